# Optimizing a Trainium2 kernel written in Bass

```python
import math
import jax, jax.numpy as jnp
from jax import lax
import numpy as np

D_MODEL = 1024
BATCH = 8
SEQ = 2048
DEPTH = 2

HEAD_DIM = 64
POOL_GROUPS = 4
POOL_WINDOWS = (2, 4, 8, 16)
POOL_GROUP_DIM = 128
POOL_WIDTH = POOL_GROUPS * POOL_GROUP_DIM
DIFF_HEADS = 4
DIFF_QK = DIFF_HEADS * 2 * HEAD_DIM
DIFF_V_DIM = 2 * HEAD_DIM
DIFF_V = DIFF_HEADS * DIFF_V_DIM
FOX_HEADS = 8
FOX_QK = FOX_HEADS * HEAD_DIM
FOX_V = FOX_HEADS * HEAD_DIM
N_BRANCH = 3
BRANCH_WIDTH = 512
ROT_DIM = HEAD_DIM // 4
ROPE_THETA = 500000.0
Q_BLOCK = 128
PLE_DIM = 256
N_GROUPS = 4
EXPERTS_PER_GROUP = 4
N_EXPERTS = N_GROUPS * EXPERTS_PER_GROUP
TOP_K = 2
D_EXPERT = 256
RMS_EPS = 1e-6
IN_SPLITS = (POOL_WIDTH, DIFF_QK, DIFF_QK, DIFF_V, FOX_QK, FOX_QK, FOX_V, FOX_HEADS, N_BRANCH * D_MODEL)
IN_WIDTH = sum(IN_SPLITS)

kernel_name = "hybrid_pool_diffattn_fox_hmoe_ple"


def rms_norm(x, g):
    x32 = x.astype(jnp.float32)
    y = x32 * lax.rsqrt(jnp.mean(x32 * x32, axis=-1, keepdims=True) + RMS_EPS)
    return (y * g.astype(jnp.float32)).astype(x.dtype)


def partial_rotary(x, cos, sin):
    half = ROT_DIM // 2
    x1 = x[..., :half]
    x2 = x[..., half:ROT_DIM]
    c = cos.astype(x.dtype)
    s = sin.astype(x.dtype)
    return jnp.concatenate([x1 * c - x2 * s, x2 * c + x1 * s, x[..., ROT_DIM:]], axis=-1)


def pool_mixer(u, w_grp, scale):
    B, S, _ = u.shape
    u = u.reshape(B, S, POOL_GROUPS, POOL_GROUP_DIM)
    u32 = u.astype(jnp.float32)
    csum = jnp.cumsum(u32, axis=1)
    t = jnp.arange(S)
    outs = []
    for g, w in enumerate(POOL_WINDOWS):
        c = csum[:, :, g]
        lag = jnp.pad(c, ((0, 0), (w, 0), (0, 0)))[:, :S]
        cnt = jnp.minimum(t + 1, w).astype(jnp.float32)[None, :, None]
        outs.append((c - lag) / cnt - u32[:, :, g])
    d = jnp.stack(outs, axis=2).astype(u.dtype)
    y = jnp.einsum('bsgc,gcd->bsgd', d, w_grp)
    return y.reshape(B, S, POOL_WIDTH) * scale


def _masked_softmax(s, mask):
    return jax.nn.softmax(jnp.where(mask, s, -jnp.inf), axis=-1)


def diff_attention(q1, q2, k1, k2, v, lam):
    S, dh = q1.shape[2], q1.shape[3]
    scale = dh ** -0.5
    outs = []
    for q0 in range(0, S, Q_BLOCK):
        kend = q0 + Q_BLOCK
        mask = jnp.arange(kend)[None, :] <= jnp.arange(q0, kend)[:, None]
        s1 = jnp.einsum('bhqd,bhkd->bhqk', q1[:, :, q0:kend], k1[:, :, :kend]).astype(jnp.float32) * scale
        s2 = jnp.einsum('bhqd,bhkd->bhqk', q2[:, :, q0:kend], k2[:, :, :kend]).astype(jnp.float32) * scale
        a = _masked_softmax(s1, mask) - lam * _masked_softmax(s2, mask)
        outs.append(jnp.einsum('bhqk,bhkd->bhqd', a.astype(v.dtype), v[:, :, :kend]))
    return jnp.concatenate(outs, axis=2)


def forgetting_attention(q, k, v, logf):
    S, dh = q.shape[2], q.shape[3]
    scale = dh ** -0.5
    F = jnp.cumsum(logf, axis=-1)
    outs = []
    for q0 in range(0, S, Q_BLOCK):
        kend = q0 + Q_BLOCK
        mask = jnp.arange(kend)[None, :] <= jnp.arange(q0, kend)[:, None]
        s = jnp.einsum('bhqd,bhkd->bhqk', q[:, :, q0:kend], k[:, :, :kend]).astype(jnp.float32) * scale
        s = s + (F[:, :, q0:kend, None] - F[:, :, None, :kend])
        a = _masked_softmax(s, mask)
        outs.append(jnp.einsum('bhqk,bhkd->bhqd', a.astype(v.dtype), v[:, :, :kend]))
    return jnp.concatenate(outs, axis=2)


def hierarchical_moe(h, w_rg, b_rg, w_re, b_re, w_gate, w_up, w_down):
    T = h.shape[0]
    g_prob = jax.nn.softmax((h @ w_rg).astype(jnp.float32) + b_rg.astype(jnp.float32), axis=-1)
    gp, gi = lax.top_k(g_prob, 1)
    e_logit = ((h @ w_re).astype(jnp.float32) + b_re.astype(jnp.float32)).reshape(T, N_GROUPS, EXPERTS_PER_GROUP)
    e_sel = jnp.take_along_axis(e_logit, gi[:, :, None], axis=1)[:, 0]
    ew, ei = lax.top_k(jax.nn.softmax(e_sel, axis=-1), TOP_K)
    weight = gp * ew / jnp.sum(ew, axis=-1, keepdims=True)
    eid = gi * EXPERTS_PER_GROUP + ei
    combine = jnp.sum(jax.nn.one_hot(eid, N_EXPERTS, dtype=jnp.float32) * weight[..., None], axis=1)
    act = jax.nn.silu(jnp.einsum('td,edf->tef', h, w_gate)) * jnp.einsum('td,edf->tef', h, w_up)
    return jnp.einsum('tef,efd->td', act * combine[:, :, None].astype(act.dtype), w_down)


def setup_inputs(seed: int = 0) -> dict:
    key = jax.random.key(seed)
    ks = jax.random.split(key, 32)
    f32 = jnp.float32
    L, D = DEPTH, D_MODEL

    def nrm(k, shape, scale):
        return jax.random.normal(k, shape, f32) * scale

    def gain(k, shape):
        return 1.0 + 0.05 * jax.random.normal(k, shape, f32)

    return {
        "x": nrm(ks[0], (BATCH, SEQ, D), 1.0),
        "p": nrm(ks[1], (L, BATCH, SEQ, PLE_DIM), 1.0),
        "positions": jnp.broadcast_to(jnp.arange(SEQ, dtype=jnp.int32), (BATCH, SEQ)),
        "attn_norm_g": gain(ks[2], (L, D)),
        "w_in": nrm(ks[3], (L, D, IN_WIDTH), D ** -0.5),
        "pool_w": nrm(ks[4], (L, POOL_GROUPS, POOL_GROUP_DIM, POOL_GROUP_DIM), POOL_GROUP_DIM ** -0.5),
        "pool_scale": gain(ks[5], (L, POOL_WIDTH)),
        "diff_qn_g": gain(ks[6], (L, HEAD_DIM)),
        "diff_kn_g": gain(ks[7], (L, HEAD_DIM)),
        "diff_lambda": nrm(ks[8], (L, 4, HEAD_DIM), 0.1),
        "diff_subln_g": gain(ks[9], (L, DIFF_V_DIM)),
        "fox_qn_g": gain(ks[10], (L, HEAD_DIM)),
        "fox_kn_g": gain(ks[11], (L, HEAD_DIM)),
        "fox_forget_b": 2.0 + 0.1 * jax.random.normal(ks[12], (L, FOX_HEADS), f32),
        "w_branch": nrm(ks[13], (L, N_BRANCH, BRANCH_WIDTH, D), BRANCH_WIDTH ** -0.5),
        "w_out": nrm(ks[14], (L, D, D), D ** -0.5),
        "ffn_norm_g": gain(ks[15], (L, D)),
        "w_route_group": nrm(ks[16], (L, D, N_GROUPS), D ** -0.5),
        "b_route_group": nrm(ks[17], (L, N_GROUPS), 0.01),
        "w_route_expert": nrm(ks[18], (L, D, N_EXPERTS), D ** -0.5),
        "b_route_expert": nrm(ks[19], (L, N_EXPERTS), 0.01),
        "moe_w_gate": nrm(ks[20], (L, N_EXPERTS, D, D_EXPERT), D ** -0.5),
        "moe_w_up": nrm(ks[21], (L, N_EXPERTS, D, D_EXPERT), D ** -0.5),
        "moe_w_down": nrm(ks[22], (L, N_EXPERTS, D_EXPERT, D), D_EXPERT ** -0.5),
        "ple_norm_g": gain(ks[23], (L, D)),
        "w_ple_gate": nrm(ks[24], (L, D, D), D ** -0.5),
        "w_ple": nrm(ks[25], (L, PLE_DIM, D), PLE_DIM ** -0.5),
    }


def reference(x, p, positions, attn_norm_g, w_in, pool_w, pool_scale, diff_qn_g, diff_kn_g, diff_lambda,
              diff_subln_g, fox_qn_g, fox_kn_g, fox_forget_b, w_branch, w_out, ffn_norm_g, w_route_group,
              b_route_group, w_route_expert, b_route_expert, moe_w_gate, moe_w_up, moe_w_down, ple_norm_g,
              w_ple_gate, w_ple):
    B, S, D = x.shape
    inv_freq = ROPE_THETA ** (-jnp.arange(0, ROT_DIM, 2, dtype=jnp.float32) / ROT_DIM)
    ang = positions.astype(jnp.float32)[:, None, :, None] * inv_freq
    cos, sin = jnp.cos(ang), jnp.sin(ang)
    split_idx = np.cumsum(IN_SPLITS)[:-1].tolist()

    for l in range(DEPTH):
        h = rms_norm(x, attn_norm_g[l])
        proj = h @ w_in[l]
        u_pool, dq, dk, dv, fq, fk, fv, fz, gz = jnp.split(proj, split_idx, axis=-1)

        y_pool = pool_mixer(u_pool, pool_w[l], pool_scale[l])

        dq = jnp.transpose(rms_norm(dq.reshape(B, S, DIFF_HEADS, 2, HEAD_DIM), diff_qn_g[l]), (0, 2, 3, 1, 4))
        dk = jnp.transpose(rms_norm(dk.reshape(B, S, DIFF_HEADS, 2, HEAD_DIM), diff_kn_g[l]), (0, 2, 3, 1, 4))
        q1, q2 = partial_rotary(dq[:, :, 0], cos, sin), partial_rotary(dq[:, :, 1], cos, sin)
        k1, k2 = partial_rotary(dk[:, :, 0], cos, sin), partial_rotary(dk[:, :, 1], cos, sin)
        dv = jnp.transpose(dv.reshape(B, S, DIFF_HEADS, DIFF_V_DIM), (0, 2, 1, 3))
        lam_init = 0.8 - 0.6 * math.exp(-0.3 * l)
        lp = diff_lambda[l].astype(jnp.float32)
        lam = jnp.exp(jnp.sum(lp[0] * lp[1])) - jnp.exp(jnp.sum(lp[2] * lp[3])) + lam_init
        o_diff = diff_attention(q1, q2, k1, k2, dv, lam)
        o_diff = rms_norm(o_diff, diff_subln_g[l]) * (1.0 - lam_init)
        y_diff = jnp.transpose(o_diff, (0, 2, 1, 3)).reshape(B, S, DIFF_V)

        fq = jnp.transpose(rms_norm(fq.reshape(B, S, FOX_HEADS, HEAD_DIM), fox_qn_g[l]), (0, 2, 1, 3))
        fk = jnp.transpose(rms_norm(fk.reshape(B, S, FOX_HEADS, HEAD_DIM), fox_kn_g[l]), (0, 2, 1, 3))
        fv = jnp.transpose(fv.reshape(B, S, FOX_HEADS, HEAD_DIM), (0, 2, 1, 3))
        logf = jnp.transpose(jax.nn.log_sigmoid(fz.astype(jnp.float32) + fox_forget_b[l].astype(jnp.float32)), (0, 2, 1))
        o_fox = forgetting_attention(fq, fk, fv, logf)
        y_fox = jnp.transpose(o_fox, (0, 2, 1, 3)).reshape(B, S, FOX_V)

        branches = jnp.stack([y_pool, y_diff, y_fox], axis=2)
        br = jnp.einsum('bsnc,ncd->bsnd', branches, w_branch[l])
        gates = jax.nn.sigmoid(gz.reshape(B, S, N_BRANCH, D))
        merged = jnp.sum(gates * br, axis=2)
        x = x + merged @ w_out[l]

        hf = rms_norm(x, ffn_norm_g[l]).reshape(B * S, D)
        y = hierarchical_moe(hf, w_route_group[l], b_route_group[l], w_route_expert[l], b_route_expert[l],
                             moe_w_gate[l], moe_w_up[l], moe_w_down[l])
        x = x + y.reshape(B, S, D)

        hp = rms_norm(x, ple_norm_g[l])
        x = x + jax.nn.sigmoid(hp @ w_ple_gate[l]) * (p[l] @ w_ple[l])
    return x
```

```python
import contextlib
import math
import numpy as np
import concourse.bass as bass
import concourse.mybir as mybir
from concourse.bass_utils import run_bass_kernel_spmd

F32 = mybir.dt.float32
BF16 = mybir.dt.bfloat16
I32 = mybir.dt.int32
AF = mybir.ActivationFunctionType
ALU = mybir.AluOpType
AX = mybir.AxisListType
ENGS = ("pe", "act", "dve", "pool", "sp")

S = 2048
D = 1024
NT = 16
NQ = 4
INW = 6664
EPS = 1e-6
BIG = 30000.0
O_POOL, O_DQ, O_DK, O_DV, O_FQ, O_FK, O_FV, O_FZ, O_GZ = 0, 512, 1024, 1536, 2048, 2560, 3072, 3584, 3592


class Graph:
    SAME_ENG_WINDOW = 10 ** 9

    def __init__(self, nc):
        self.nc = nc
        self.ops = []
        self.last_w = {}
        self.readers = {}
        self.stream = {e: [] for e in ENGS}
        self.dma_count = {}

    def _add(self, op, r, w):
        deps = set()
        for k in r:
            if k in self.last_w:
                deps.add(self.last_w[k])
        for k in w:
            if k in self.last_w:
                deps.add(self.last_w[k])
            for x in self.readers.get(k, ()):
                deps.add(x)
        idx = len(self.ops)
        op["deps"] = deps
        op["idx"] = idx
        op["signal"] = False
        self.ops.append(op)
        op["pos"] = len(self.stream[op["eng"]])
        self.stream[op["eng"]].append(idx)
        for k in w:
            self.last_w[k] = idx
            self.readers[k] = []
        for k in r:
            if k not in w:
                self.readers.setdefault(k, []).append(idx)
        return idx

    def add(self, eng, fn, r=(), w=()):
        return self._add({"eng": eng, "fn": fn, "kind": "c"}, tuple(r), tuple(w))

    def dma(self, eng, out, in_, r=(), w=(), slot=None, nfill=1, cont=False):
        if not cont:
            self.dma_count[slot] = self.dma_count.get(slot, 0) + nfill
        n = self.dma_count[slot]
        idx = self._add({"eng": eng, "kind": "d", "out": out, "in": in_, "slot": slot, "n": n},
                        tuple(r), tuple(w))
        op = self.ops[idx]
        op["deps"] = {d for d in op["deps"]
                      if not (self.ops[d]["kind"] == "d" and self.ops[d]["slot"] == slot and self.ops[d]["n"] == n)}
        return idx

    def emit(self):
        nc = self.nc
        ops = self.ops
        waits = [None] * len(ops)
        for e in ENGS:
            kn = {}
            for idx in self.stream[e]:
                op = ops[idx]
                need = {}
                for d in op["deps"]:
                    Dp = ops[d]
                    if Dp["kind"] == "d":
                        key = ("d", Dp["slot"])
                        val = Dp["n"]
                    else:
                        if Dp["eng"] == e:
                            if e == "pe" or op["pos"] - Dp["pos"] > self.SAME_ENG_WINDOW:
                                continue
                        key = ("e", Dp["eng"])
                        val = Dp["pos"]
                    if kn.get(key, -1) >= val:
                        continue
                    if need.get(key, (-1, None))[0] < val:
                        need[key] = (val, d)
                for key, (val, d) in need.items():
                    kn[key] = val
                    ops[d]["signal"] = True
                waits[idx] = [d for (_, d) in need.values()]
        cnt = {e: 0 for e in ENGS}
        for e in ENGS:
            for idx in self.stream[e]:
                op = ops[idx]
                if op["kind"] == "c" and op["signal"]:
                    cnt[e] += 1
                    op["val"] = cnt[e]
        self.counts = cnt
        with contextlib.ExitStack() as es:
            esem = {e: es.enter_context(nc.semaphore("c_" + e)) for e in ENGS}
            dsem = {}
            for s in self.dma_count:
                dsem[s] = es.enter_context(nc.semaphore("d_%d" % len(dsem)))
            block = es.enter_context(nc.Block())

            def run(e, eng):
                last = {}
                for idx in self.stream[e]:
                    op = ops[idx]
                    for d in waits[idx]:
                        Dp = ops[d]
                        if Dp["kind"] == "d":
                            eng.wait_ge(dsem[Dp["slot"]], 16 * Dp["n"])
                        else:
                            eng.wait_ge(esem[Dp["eng"]], Dp["val"])
                    if op["kind"] == "d":
                        eng.dma_start(out=op["out"], in_=op["in"]).then_inc(dsem[op["slot"]], 16)
                        last[op["slot"]] = max(last.get(op["slot"], 0), op["n"])
                    else:
                        ins = op["fn"](eng)
                        if op["signal"]:
                            ins.then_inc(esem[e], 1)
                for s, n in last.items():
                    eng.wait_ge(dsem[s], 16 * n)

            @block.tensor
            def _(eng):
                run("pe", eng)

            @block.scalar
            def _(eng):
                run("act", eng)

            @block.vector
            def _(eng):
                run("dve", eng)

            @block.gpsimd
            def _(eng):
                run("pool", eng)

            @block.sync
            def _(eng):
                run("sp", eng)


def build_program(NL, layer0):
    nc = bass.Bass("TRN2", target_bir_lowering=False)

    def din(name, shape, dt=F32):
        return nc.dram_tensor(name, list(shape), dt, kind="ExternalInput").ap()

    x_d = din("x", [S, D])
    p_d = din("p", [NL, S, 256])
    pos_d = din("positions", [NT, 128], I32)
    attn_g_d = din("attn_norm_g", [NL, D])
    w_in_d = din("w_in", [NL, D, INW])
    pool_w_d = din("pool_w", [NL, 4, 128, 128])
    pool_s_d = din("pool_scale", [NL, 512])
    dqg_d = din("diff_qn_g", [NL, 64])
    dkg_d = din("diff_kn_g", [NL, 64])
    dlam_d = din("diff_lambda", [NL, 256])
    dsub_d = din("diff_subln_g", [NL, 128])
    fqg_d = din("fox_qn_g", [NL, 64])
    fkg_d = din("fox_kn_g", [NL, 64])
    ffb_d = din("fox_forget_b", [NL, 8])
    w_br_d = din("w_branch", [NL, 3, 512, D])
    w_out_d = din("w_out", [NL, D, D])
    ffn_g_d = din("ffn_norm_g", [NL, D])
    wrg_d = din("w_route_group", [NL, D, 4])
    brg_d = din("b_route_group", [NL, 4])
    wre_d = din("w_route_expert", [NL, D, 16])
    bre_d = din("b_route_expert", [NL, 16])
    mg_d = din("moe_w_gate", [NL, 16, D, 256])
    mu_d = din("moe_w_up", [NL, 16, D, 256])
    md_d = din("moe_w_down", [NL, 16, 256, D])
    ple_g_d = din("ple_norm_g", [NL, D])
    wpg_d = din("w_ple_gate", [NL, D, D])
    wp_d = din("w_ple", [NL, 256, D])
    out_d = nc.dram_tensor("out", [S, D], F32, kind="ExternalOutput").ap()
    comb_scr = nc.dram_tensor("comb_scr", [16, S], F32, kind="Internal").ap()

    es = contextlib.ExitStack()
    with es:
        def sb(name, shape, dt):
            return es.enter_context(nc.sbuf_tensor(name, list(shape), dt))

        def ps(name, shape, dt):
            return es.enter_context(nc.psum_tensor(name, list(shape), dt))

        G = Graph(nc)
        A = G.add

        x_sb = sb("x_sb", [128, NT, D], F32)
        hT = sb("hT", [128, 8, S], BF16)
        wsl = [sb("wsl%d" % i, [128, 4096], BF16) for i in range(2)]
        yT = sb("yT", [128, 12, S], BF16)
        R16 = sb("R16", [128, 8192], BF16)
        PR = sb("PR", [128, 8, 512], BF16)
        TFa = sb("TFa", [128, 4, 512], F32)
        TF = [TFa[:, i, :] for i in range(4)]
        TB = [sb("TB%d" % i, [128, 1024], BF16) for i in range(2)]
        g_bc = TFa[:, 0:2, :].rearrange("p a c -> p (a c)")
        ident_f = sb("ident_f", [128, 128], F32)
        ones_f = sb("ones_f", [128, 128], F32)
        tri_f = sb("tri_f", [128, 128], F32)
        sel_last = sb("sel_last", [128, 128], F32)
        ident_b = sb("ident_b", [128, 128], BF16)
        ones_b = sb("ones_b", [128, 128], BF16)
        tri_b = sb("tri_b", [128, 128], BF16)
        eps_t = sb("eps_t", [128, 1], F32)
        one_t = sb("one_t", [128, 1], F32)
        pi_t = sb("pi_t", [128, 1], F32)
        zero_t = sb("zero_t", [128, 1], F32)
        cos_t = sb("cos_t", [128, NT, 8], F32)
        sin_t = sb("sin_t", [128, NT, 8], F32)
        sm = sb("sm", [128, 96], F32)
        qkg = [sb("qkg%d" % i, [128, 256], F32) for i in range(2)]
        ffb_bc = sb("ffb_bc", [128, 8], F32)
        Gc = sb("Gc", [128, NT, 8], F32)
        Gend = sb("Gend", [128, NT, 8], F32)
        zz = TFa[:, 2, 0:128].rearrange("p (t c) -> p t c", c=8)
        carry = TFa[:, 2, 128:256].rearrange("p (t c) -> p t c", c=8)
        biasu = sb("biasu", [128, 2, NT, NT], F32)
        wfz = sb("wfz", [128, 8, 8], BF16)
        poolw = sb("poolw", [128, 4, 128], BF16)
        pools = sb("pools", [128, 4], F32)
        ic16 = sb("ic16", [128, 4, 16], F32)
        lamt = TFa[:, 3, 0:256]
        lam4 = sb("lam4", [128, 8], F32)
        subg = sb("subg", [128, 1], F32)
        w_r = sb("w_r", [128, 8, 20], BF16)
        b_r = sb("b_r", [128, 20], F32)
        TB1f = TB[1][:].bitcast(F32)
        LG = TB1f[:, 0:64].rearrange("p (t c) -> p t c", c=4)
        LE = TFa[:, 2, 0:256].rearrange("p (t c) -> p t c", c=16)
        RA = TFa[:, 2, 256:512].rearrange("p (t c) -> p t c", c=16)
        RB = TFa[:, 3, 0:256].rearrange("p (t c) -> p t c", c=16)
        RS = TB1f[:, 128:256].rearrange("p (a t) -> p a t", a=8)
        RP = TB1f[:, 64:128].rearrange("p (t c) -> p t c", c=4)
        comb = TFa[:, 3, 256:512].rearrange("p (t c) -> p t c", c=16)
        halo = sb("halo", [128, 16], F32)
        PSa = ps("PSa", [128, 8, 512], F32)
        pb = [PSa[:, i, :] for i in range(8)]

        def pbb(i):
            return pb[i].bitcast(BF16)

        R16f = R16[:].bitcast(F32)
        tsl = lambda t: slice(t * 128, (t + 1) * 128)
        qsl = lambda q: slice(q * 512, (q + 1) * 512)

        fence_t = sb("fence_t", [128, 1], F32)

        def fence(old, new):
            A("dve", lambda e: e.memset(fence_t[:], 0.0), w=list(old) + list(new) + ["fence_t"])

        A("pool", lambda e: e.memset(ones_f[:], 1.0), w=["ones_f"])
        A("pool", lambda e: e.memset(ones_b[:], 1.0), w=["ones_b"])
        A("pool", lambda e: e.memset(eps_t[:], EPS), w=["eps_t"])
        A("pool", lambda e: e.memset(one_t[:], 1.0), w=["one_t"])
        A("pool", lambda e: e.memset(pi_t[:], math.pi), w=["pi_t"])
        A("pool", lambda e: e.memset(zero_t[:], 0.0), w=["zero_t"])
        A("pool", lambda e: e.affine_select(ident_f[:], ones_f[:], [[1, 128]], ALU.is_equal, 0.0, base=0, channel_multiplier=-1),
          r=["ones_f"], w=["ident_f"])
        A("pool", lambda e: e.affine_select(tri_f[:], ones_f[:], [[1, 128]], ALU.is_ge, 0.0, base=0, channel_multiplier=-1),
          r=["ones_f"], w=["tri_f"])
        A("pool", lambda e: e.affine_select(sel_last[:], ones_f[:], [[0, 128]], ALU.is_equal, 0.0, base=-127, channel_multiplier=1),
          r=["ones_f"], w=["sel_last"])
        A("pool", lambda e: e.tensor_copy(ident_b[:], ident_f[:]), r=["ident_f"], w=["ident_b"])
        A("pool", lambda e: e.tensor_copy(tri_b[:], tri_f[:]), r=["tri_f"], w=["tri_b"])
        A("pool", lambda e: e.iota(ic16[:], [[0, 4], [1, 16]], base=1, channel_multiplier=0, allow_small_or_imprecise_dtypes=True), w=["ic16"])
        for g in range(4):
            A("dve", lambda e, g=g: e.tensor_scalar(ic16[:, g, :], ic16[:, g, :], float(2 ** (g + 1)), None, ALU.min), r=["ic16"], w=["ic16"])
        A("dve", lambda e: e.reciprocal(ic16[:], ic16[:]), r=["ic16"], w=["ic16"])

        xv = x_d.rearrange("(t p) d -> p t d", p=128)
        for i, (t0, t1) in enumerate([(0, 1), (1, 2), (2, 4), (4, 8), (8, 12), (12, 16)]):
            G.dma("sp", x_sb[:, t0:t1, :], xv[:, t0:t1, :], w=[("x", t) for t in range(t0, t1)], slot=("xl", i))

        def build_rotary():
            fence([("TB", 0), ("TB", 1)], ["posi", "posf", "ang", "angi", "angr"])
            TB0f = TB[0][:].bitcast(F32)
            TB1i = TB[1][:].bitcast(I32)
            posi = TB1i[0:NT, 0:128]
            angi = TB1i[:, 128:256].rearrange("p (t c) -> p t c", c=8)
            posf = TB0f[0:NT, 0:128]
            ang = TB0f[:, 128:256].rearrange("p (t c) -> p t c", c=8)
            angr = TB0f[:, 256:384].rearrange("p (t c) -> p t c", c=8)
            post = sb("post", [128, NT], F32)
            invf = sb("invf", [128, 8], F32)
            G.dma("sp", posi, pos_d, w=["posi"], slot="posi")
            A("dve", lambda e: e.tensor_copy(posf, posi), r=["posi"], w=["posf"])
            A("pe", lambda e: e.transpose(pb[0][:, 0:NT], posf, ident_f[0:NT, 0:NT]), r=["posf", "ident_f"], w=[("pb", 0)])
            A("dve", lambda e: e.tensor_copy(post[:], pb[0][:, 0:NT]), r=[("pb", 0)], w=["post"])
            A("pool", lambda e: e.iota(invf[:], [[1, 8]], base=0, channel_multiplier=0, allow_small_or_imprecise_dtypes=True), w=["invf"])
            A("act", lambda e: e.activation(invf[:], invf[:], AF.Exp, scale=-math.log(500000.0) / 8.0), r=["invf"], w=["invf"])
            A("dve", lambda e: e.tensor_tensor(ang, post[:].unsqueeze(2).to_broadcast([128, NT, 8]),
                                               invf[:].unsqueeze(1).to_broadcast([128, NT, 8]), ALU.mult), r=["post", "invf"], w=["ang"])

            def sin_of(dst, shift, tag):
                A("dve", lambda e: e.tensor_scalar(angr, ang, shift, 1.0 / (2 * math.pi), ALU.add, ALU.mult), r=["ang"], w=["angr"])
                A("dve", lambda e: e.tensor_copy(angi, angr), r=["angr"], w=["angi"])
                A("dve", lambda e: e.tensor_copy(angr, angi), r=["angi"], w=["angr"])
                A("dve", lambda e: e.tensor_scalar(angr, angr, -2 * math.pi, shift, ALU.mult, ALU.add), r=["angr"], w=["angr"])
                A("dve", lambda e: e.tensor_tensor(angr, angr, ang, ALU.add), r=["angr", "ang"], w=["angr"])
                A("dve", lambda e: e.tensor_scalar(angr, angr, math.pi, -math.pi, ALU.min, ALU.max), r=["angr"], w=["angr"])
                A("act", lambda e: e.activation(dst[:], angr, AF.Sin), r=["angr"], w=[tag])

            sin_of(sin_t, 0.0, "sin_t")
            sin_of(cos_t, math.pi / 2, "cos_t")

            fence(["posi", "posf", "ang", "angi", "angr"], [("TB", 0), ("TB", 1)])


        sn = sb("sn", [128, 48], F32)

        def rmsnorm_to_hT(g_dram_row):
            fence(["TF0", "TF1", "TF2", "TF3"], ["g_bc", "nj0", "nj1"])
            G.dma("sp", g_bc, g_dram_row.partition_broadcast(128), w=["g_bc"], slot="g_bc")
            junk = [TFa[:, 2, :].bitcast(BF16), TFa[:, 3, :].bitcast(BF16)]

            def s1(t):
                jk = junk[t % 2]
                kj = "nj%d" % (t % 2)
                A("act", lambda e, t=t, jk=jk: e.activation(jk, x_sb[:, t, :], AF.Square, accum_out=sn[:, t:t + 1]), r=[("x", t)], w=[kj, ("sn", t)])
                A("act", lambda e, t=t: e.activation(sn[:, 16 + t:17 + t], sn[:, t:t + 1], AF.Ln, bias=eps_t[:], scale=1.0 / D), r=[("sn", t), "eps_t"], w=[("sn1", t)])
                A("act", lambda e, t=t: e.activation(sn[:, 32 + t:33 + t], sn[:, 16 + t:17 + t], AF.Exp, scale=-0.5), r=[("sn1", t)], w=[("sn2", t)])

            def s2a(t):
                tb = TB[t % 2]
                kt = ("TB", t % 2)
                bank = t % 2
                A("dve", lambda e, t=t, tb=tb: e.scalar_tensor_tensor(tb[:], x_sb[:, t, :], sn[:, 32 + t:33 + t], g_bc, ALU.mult, ALU.mult),
                  r=[("x", t), ("sn2", t), "g_bc"], w=[kt])
                for k in range(8):
                    A("pe", lambda e, k=k, tb=tb, bank=bank: e.transpose(pbb(bank)[:, k * 128:(k + 1) * 128], tb[:, k * 128:(k + 1) * 128], ident_b[:]),
                      r=[kt, "ident_b"], w=[("pb", bank)])

            def s2b(t):
                bank = t % 2
                A("dve", lambda e, t=t, bank=bank: e.tensor_copy(hT[:, :, tsl(t)], pbb(bank).rearrange("p (k c) -> p k c", k=8)),
                  r=[("pb", bank)], w=[("hT", t)])

            s1(0)
            s1(1)
            for t in range(NT):
                s2a(t)
                if t + 2 < NT:
                    s1(t + 2)
                if t >= 1:
                    s2b(t - 1)
            s2b(NT - 1)
            fence(["g_bc", "nj0", "nj1"], ["TF0", "TF1", "TF2", "TF3"])

        K_PR = [("PR", i) for i in range(8)]
        K_POOLST = ["U0_0", "U1_0", "U2_0", "DB_0", "U0_1", "U1_1", "U2_1", "DB_1"]
        K_UNIT = [("QK", t) for t in range(NT)] + [("Vu", t) for t in range(NT)]
        K_M4 = [("M4", dc, q) for dc in range(4) for q in range(NQ)]
        K_MOE16 = ["combT", "selE"]
        K_PLE16 = ["PB"] + [("PT", t) for t in range(NT)]
        K_YT = [("yT", c, q) for c in range(12) for q in range(NQ)] + [("yTb", c, q) for c in range(8, 12) for q in range(NQ)]
        K_MOEY = [("ACT4", c, q) for c in range(8) for q in range(NQ)] + [("WD", e4) for e4 in range(4)]

        slot_i = [0]

        def next_slot():
            i = slot_i[0] % 2
            slot_i[0] += 1
            return i

        for li in range(NL):
            lglob = layer0 + li
            lam_init = 0.8 - 0.6 * math.exp(-0.3 * lglob)
            w_in_v = w_in_d[li].rearrange("(k p) c -> p k c", p=128)

            rmsnorm_to_hT(attn_g_d[li])
            if li == 0:
                build_rotary()
            hT_all = [("hT", t) for t in range(NT)]

            G.dma("sp", qkg[0][:, 0:64], dqg_d[li].partition_broadcast(128), w=["qkg0"], slot="qkg0", nfill=4)
            G.dma("sp", qkg[0][:, 64:128], dqg_d[li].partition_broadcast(128), w=["qkg0"], slot="qkg0", cont=True)
            G.dma("sp", qkg[0][:, 128:192], dkg_d[li].partition_broadcast(128), w=["qkg0"], slot="qkg0", cont=True)
            G.dma("sp", qkg[0][:, 192:256], dkg_d[li].partition_broadcast(128), w=["qkg0"], slot="qkg0", cont=True)
            G.dma("sp", qkg[1][:, 0:64], fqg_d[li].partition_broadcast(128), w=["qkg1"], slot="qkg1", nfill=4)
            G.dma("sp", qkg[1][:, 64:128], fqg_d[li].partition_broadcast(128), w=["qkg1"], slot="qkg1", cont=True)
            G.dma("sp", qkg[1][:, 128:192], fkg_d[li].partition_broadcast(128), w=["qkg1"], slot="qkg1", cont=True)
            G.dma("sp", qkg[1][:, 192:256], fkg_d[li].partition_broadcast(128), w=["qkg1"], slot="qkg1", cont=True)
            G.dma("sp", ffb_bc[:], ffb_d[li].partition_broadcast(128), w=["ffb"], slot="ffb")
            fence(["TF3"], ["lamt"])
            G.dma("sp", lamt, dlam_d[li].partition_broadcast(128), w=["lamt"], slot="lamt")
            G.dma("sp", subg[:], dsub_d[li].rearrange("(p o) -> p o", o=1), w=["subg"], slot="subg")
            G.dma("sp", pools[:], pool_s_d[li].rearrange("(g p) -> p g", p=128), w=["pools"], slot="pools")
            G.dma("pool", poolw[:], pool_w_d[li].rearrange("g c d -> c g d"), w=["poolw"], slot="poolw")
            G.dma("pool", wfz[:], w_in_v[:, :, O_FZ:O_FZ + 8], w=["wfz"], slot="wfz")
            A("dve", lambda e: e.tensor_tensor(lamt[:, 0:64], lamt[:, 0:64], lamt[:, 64:128], ALU.mult), r=["lamt"], w=["lamt"])
            A("dve", lambda e: e.tensor_tensor(lamt[:, 128:192], lamt[:, 128:192], lamt[:, 192:256], ALU.mult), r=["lamt"], w=["lamt"])
            A("dve", lambda e: e.tensor_reduce(lam4[:, 0:1], lamt[:, 0:64], AX.X, ALU.add), r=["lamt"], w=["lam4"])
            A("dve", lambda e: e.tensor_reduce(lam4[:, 1:2], lamt[:, 128:192], AX.X, ALU.add), r=["lamt"], w=["lam4"])
            A("act", lambda e: e.activation(lam4[:, 2:4], lam4[:, 0:2], AF.Exp), r=["lam4"], w=["lam4"])
            A("dve", lambda e: e.tensor_tensor(lam4[:, 4:5], lam4[:, 3:4], lam4[:, 2:3], ALU.subtract), r=["lam4"], w=["lam4"])
            A("dve", lambda e, li_=lam_init: e.tensor_scalar(lam4[:, 5:6], lam4[:, 4:5], -li_, None, ALU.add), r=["lam4"], w=["lam4"])
            A("dve", lambda e, li_=lam_init: e.tensor_scalar(subg[:], subg[:], 1.0 - li_, None, ALU.mult), r=["subg"], w=["subg"])
            fence(["lamt"], ["TF3"])

            fence(["TF2"], ["zz", "carry"])
            for t in range(NT):
                for k in range(8):
                    A("pe", lambda e, t=t, k=k: e.matmul(pb[2][:, t * 8:(t + 1) * 8], hT[:, k, tsl(t)], wfz[:, k, :], start=(k == 0), stop=(k == 7)),
                      r=[("hT", t), "wfz"], w=[("pb", 2)])
            A("dve", lambda e: e.tensor_tensor(zz, pb[2][:, 0:128].rearrange("p (t h) -> p t h", h=8),
                                               ffb_bc[:].unsqueeze(1).to_broadcast([128, NT, 8]), ALU.add), r=[("pb", 2), "ffb"], w=["zz"])
            A("act", lambda e: e.activation(zz, zz, AF.Exp, scale=-1.0), r=["zz"], w=["zz"])
            A("act", lambda e: e.activation(zz, zz, AF.Ln, bias=one_t[:]), r=["zz", "one_t"], w=["zz"])
            zz2 = TFa[:, 2, 0:128]
            A("pe", lambda e: e.matmul(pb[3][:, 0:128], tri_f[:], zz2, start=True, stop=True), r=["zz", "tri_f"], w=[("pb", 3)])
            A("pe", lambda e: e.matmul(pb[3][:, 128:256], ones_f[:], zz2, start=True, stop=True), r=["zz", "ones_f"], w=[("pb", 3)])
            A("dve", lambda e: e.memset(carry[:, 0, :], 0.0), w=["carry"])
            A("dve", lambda e: e.tensor_copy(Gend[:], pb[3][:, 128:256].rearrange("p (t h) -> p t h", h=8)), r=[("pb", 3)], w=["Gend"])
            for t in range(1, NT):
                A("dve", lambda e, t=t: e.tensor_tensor(carry[:, t, :], carry[:, t - 1, :], Gend[:, t - 1, :], ALU.add), r=["carry", "Gend"], w=["carry"])
            A("dve", lambda e: e.tensor_tensor(Gc[:], pb[3][:, 0:128].rearrange("p (t h) -> p t h", h=8), carry, ALU.add),
              r=[("pb", 3), "carry"], w=["Gc"])
            A("pe", lambda e: e.matmul(pb[3][:, 256:384], sel_last[:], Gc[:].rearrange("p t h -> p (t h)"), start=True, stop=True),
              r=["Gc", "sel_last"], w=[("pb", 3)])
            A("dve", lambda e: e.tensor_copy(Gend[:], pb[3][:, 256:384].rearrange("p (t h) -> p t h", h=8)), r=[("pb", 3)], w=["Gend"])

            fence(["zz", "carry"], ["TF2"])
            if li > 0:
                fence(K_PLE16, K_POOLST)
                fence(K_MOEY, K_YT)
                fence(["WP"], K_PR)
            si = next_slot()
            wk = ("wsl", si)
            G.dma("pool", wsl[si][:].rearrange("p (k c) -> p k c", k=8), w_in_v[:, :, O_POOL:O_POOL + 512], w=[wk], slot=wk)
            wv = wsl[si][:].rearrange("p (k c) -> p k c", k=8)
            US = [[R16f[:, (st * 3 + i) * 528:(st * 3 + i + 1) * 528] for i in range(3)] for st in range(2)]
            DBS = [R16[:, 6400 + st * 512:6400 + (st + 1) * 512] for st in range(2)]
            for st in range(2):
                A("dve", lambda e, st=st: e.memset(US[st][1], 0.0), w=["U1_%d" % st])
                A("dve", lambda e, st=st: e.memset(US[st][2], 0.0), w=["U2_%d" % st])
            items = [(g, q) for g in range(4) for q in range(NQ)]

            def pool_M(i):
                g, q = items[i]
                bank = 4 + i % 2
                for k in range(8):
                    A("pe", lambda e, g=g, q=q, k=k, bank=bank, wv=wv: e.matmul(pb[bank], wv[:, k, g * 128:(g + 1) * 128], hT[:, k, qsl(q)], start=(k == 0), stop=(k == 7)),
                      r=[wk] + hT_all[4 * q:4 * q + 4], w=[("pb", bank)])

            def pool_CH(i):
                g, q = items[i]
                st = i % 2
                U = US[st]
                DB = DBS[st]
                ku = ["U0_%d" % st, "U1_%d" % st, "U2_%d" % st]
                kd = "DB_%d" % st
                wwin = 2 ** (g + 1)
                bank = 4 + i % 2
                if q == 0:
                    A("pool", lambda e, U=U: e.memset(U[0][:, 0:16], 0.0), w=[ku[0]])
                else:
                    A("pool", lambda e, U=U: e.tensor_copy(U[0][:, 0:16], halo[:]), r=["halo"], w=[ku[0]])
                A("act", lambda e, bank=bank, U=U: e.copy(U[0][:, 16:528], pb[bank]), r=[("pb", bank)], w=[ku[0]])
                A("pool", lambda e, U=U: e.tensor_copy(halo[:], U[0][:, 512:528]), r=[ku[0]], w=["halo"])
                src, srck = U[0], ku[0]
                sh = 1
                for step in range(g + 1):
                    dst, dstk = (U[1], ku[1]) if step % 2 == 0 else (U[2], ku[2])
                    A("dve", lambda e, src=src, dst=dst, sh=sh: e.tensor_tensor(dst[:, sh:528], src[:, sh:528], src[:, 0:528 - sh], ALU.add),
                      r=[srck], w=[dstk])
                    src, srck = dst, dstk
                    sh *= 2
                if q == 0:
                    A("dve", lambda e, src=src, g=g: e.tensor_tensor(src[:, 16:32], src[:, 16:32], ic16[:, g, :], ALU.mult), r=[srck, "ic16"], w=[srck])
                    A("dve", lambda e, src=src, U=U, DB=DB: e.tensor_tensor(DB[:, 0:16], src[:, 16:32], U[0][:, 16:32], ALU.subtract), r=[srck, ku[0]], w=[kd])
                    A("dve", lambda e, src=src, wwin=wwin, U=U, DB=DB: e.scalar_tensor_tensor(DB[:, 16:512], src[:, 32:528], 1.0 / wwin, U[0][:, 32:528], ALU.mult, ALU.subtract),
                      r=[srck, ku[0]], w=[kd])
                else:
                    A("dve", lambda e, src=src, wwin=wwin, U=U, DB=DB: e.scalar_tensor_tensor(DB[:, 0:512], src[:, 16:528], 1.0 / wwin, U[0][:, 16:528], ALU.mult, ALU.subtract),
                      r=[srck, ku[0]], w=[kd])

            def pool_Y(i):
                g, q = items[i]
                st = i % 2
                DB = DBS[st]
                yb = 6 + i % 2
                A("pe", lambda e, g=g, yb=yb, DB=DB: e.matmul(pb[yb], poolw[:, g, :], DB[:, 0:512], start=True, stop=True), r=["DB_%d" % st, "poolw"], w=[("pb", yb)])
                A("act", lambda e, g=g, q=q, yb=yb: e.activation(yT[:, g, qsl(q)], pb[yb], AF.Copy, scale=pools[:, g:g + 1]),
                  r=[("pb", yb), "pools"], w=[("yT", g, q)])

            pool_M(0)
            pool_CH(0)
            for i in range(len(items)):
                if i + 1 < len(items):
                    pool_M(i + 1)
                    pool_CH(i + 1)
                pool_Y(i)

            fence(K_POOLST, K_UNIT)
            QK = R16[:, 0:4096].rearrange("p (a s) -> p a s", a=2)
            Vu = R16[:, 4096:8192].rearrange("p (t c) -> p t c", t=NT)
            prc = [0]

            def next_pr():
                i = prc[0] % 8
                prc[0] += 1
                return i

            for u in range(8):
                is_fox = u >= 4
                j = u % 4
                oq, ok_, ov = (O_FQ, O_FK, O_FV) if is_fox else (O_DQ, O_DK, O_DV)
                si = next_slot()
                wk = ("wsl", si)
                wv = wsl[si][:, 0:3072].rearrange("p (k c) -> p k c", k=8)
                G.dma("pool", wv[:, :, 0:128], w_in_v[:, :, oq + j * 128:oq + (j + 1) * 128], w=[wk], slot=wk, nfill=3)
                G.dma("pool", wv[:, :, 128:256], w_in_v[:, :, ok_ + j * 128:ok_ + (j + 1) * 128], w=[wk], slot=wk, cont=True)
                G.dma("pool", wv[:, :, 256:384], w_in_v[:, :, ov + j * 128:ov + (j + 1) * 128], w=[wk], slot=wk, cont=True)
                gk = "qkg1" if is_fox else "qkg0"
                gt = qkg[1] if is_fox else qkg[0]
                if u == 4:
                    A("dve", lambda e: e.memset(Vu[:, :, 64:192], 1.0), w=[("Vu", t) for t in range(NT)])
                if is_fox:
                    for hh in range(2):
                        h = 2 * j + hh
                        A("dve", lambda e, hh=hh, h=h: e.tensor_tensor(biasu[:, hh, :, 0:8], Gc[:, :, h].unsqueeze(2).to_broadcast([128, NT, 8]),
                                                                      Gend[:].rearrange("p (a b) h -> p a b h", b=2)[:, :, 1, h].unsqueeze(1).to_broadcast([128, NT, 8]), ALU.subtract),
                          r=["Gc", "Gend"], w=["biasu"])
                SQ = TFa[:, 0:2, :].rearrange("p a c -> p (a c)")
                QN = TFa[:, 2:4, :].rearrange("p a c -> p (a c)")
                def b2_geom(tg):
                    s4 = 4 * (tg % 2)
                    return s4, [s4, s4 + 1], s4 + 2, s4 + 3, list(range(4 * tg, 4 * tg + 4)), TB[tg % 2], ("TB", tg % 2)

                def emit_Mqk(tg):
                    s4, bQ, bV, bT, tiles, tb, kt = b2_geom(tg)
                    for tl, t in enumerate(tiles):
                        for k in range(8):
                            A("pe", lambda e, t=t, k=k, tl=tl, bQ=bQ, wv=wv: e.matmul(pb[bQ[tl // 2]][:, (tl % 2) * 256:(tl % 2) * 256 + 256], hT[:, k, tsl(t)], wv[:, k, 0:256], start=(k == 0), stop=(k == 7)),
                              r=[("hT", t), wk], w=[("pb", bQ[tl // 2])])

                def emit_Mv(tg):
                    s4, bQ, bV, bT, tiles, tb, kt = b2_geom(tg)
                    for tl, t in enumerate(tiles):
                        for k in range(8):
                            A("pe", lambda e, t=t, k=k, tl=tl, bV=bV, wv=wv: e.matmul(pb[bV][:, tl * 128:(tl + 1) * 128], hT[:, k, tsl(t)], wv[:, k, 256:384], start=(k == 0), stop=(k == 7)),
                              r=[("hT", t), wk], w=[("pb", bV)])

                PRf = PR[:].rearrange("p a c -> p (a c)").bitcast(F32)

                def emit_CH(tg):
                    s4, bQ, bV, bT, tiles, tb, kt = b2_geom(tg)
                    if tg % 2 == 0:
                        SQb = TFa[:, 0, :].bitcast(BF16)
                        kSQ = ["TF0"]
                        QNx = TFa[:, 2:4, :].rearrange("p a c -> p (a c)")
                        kQN = ["TF2", "TF3"]
                        R4 = TFa[:, 1, :]
                        kR4 = ["TF1"]
                        so = 0
                    else:
                        SQb = PR[:, 0:2, :].rearrange("p a c -> p (a c)")
                        kSQ = [("PR", 0), ("PR", 1)]
                        QNx = PRf[:, 1024:2048]
                        kQN = [("PR", 4), ("PR", 5), ("PR", 6), ("PR", 7)]
                        R4 = PRf[:, 512:1024]
                        kR4 = [("PR", 2), ("PR", 3)]
                        so = 48
                    ka, kb_, kc = "sm_a%d" % so, "sm_b%d" % so, "sm_c%d" % so
                    PQ = PSa[:, s4:s4 + 2, :].rearrange("p b c -> p (b c)")
                    kq = [("pb", bQ[0]), ("pb", bQ[1])]
                    A("act", lambda e, PQ=PQ, SQb=SQb: e.activation(SQb, PQ, AF.Square), r=kq, w=kSQ)
                    A("dve", lambda e, SQb=SQb, so=so: e.tensor_reduce(sm[:, so:so + 16], SQb.rearrange("p (h d) -> p h d", d=64), AX.X, ALU.add), r=kSQ, w=[ka])
                    A("act", lambda e, so=so: e.activation(sm[:, so + 16:so + 32], sm[:, so:so + 16], AF.Ln, bias=eps_t[:], scale=1.0 / 64), r=[ka, "eps_t"], w=[kb_])
                    A("act", lambda e, so=so: e.activation(sm[:, so + 32:so + 48], sm[:, so + 16:so + 32], AF.Exp, scale=-0.5), r=[kb_], w=[kc])
                    A("dve", lambda e, PQ=PQ, QNx=QNx, so=so: e.tensor_tensor(QNx.rearrange("p (h d) -> p h d", d=64), PQ.rearrange("p (h d) -> p h d", d=64),
                                                                               sm[:, so + 32:so + 48].unsqueeze(2).to_broadcast([128, 16, 64]), ALU.mult), r=kq + [kc], w=kQN)
                    gb = gt[:].unsqueeze(1).to_broadcast([128, 4, 256])
                    if is_fox:
                        A("dve", lambda e, tb=tb, gb=gb, QNx=QNx: e.tensor_tensor(tb[:].rearrange("p (t c) -> p t c", t=4), QNx.rearrange("p (t c) -> p t c", t=4), gb, ALU.mult),
                          r=kQN + [gk], w=[kt])
                    else:
                        A("dve", lambda e, gb=gb, QNx=QNx: e.tensor_tensor(QNx.rearrange("p (t c) -> p t c", t=4), QNx.rearrange("p (t c) -> p t c", t=4), gb, ALU.mult),
                          r=kQN + [gk], w=kQN)
                        A("act", lambda e, tb=tb, QNx=QNx: e.copy(tb[:], QNx), r=kQN, w=[kt])
                        q4 = QNx.rearrange("p (t h d) -> p t h d", t=4, h=4)
                        o4 = tb[:].rearrange("p (t h d) -> p t h d", t=4, h=4)
                        r4 = R4.rearrange("p (a t h d) -> p a t h d", a=4, t=4, h=4)
                        cb = cos_t[:, 4 * tg:4 * tg + 4, :].unsqueeze(2).to_broadcast([128, 4, 4, 8])
                        sbb = sin_t[:, 4 * tg:4 * tg + 4, :].unsqueeze(2).to_broadcast([128, 4, 4, 8])
                        A("dve", lambda e, q4=q4, r4=r4, cb=cb: e.tensor_tensor(r4[:, 0], q4[:, :, :, 0:8], cb, ALU.mult), r=kQN + ["cos_t"], w=kR4)
                        A("dve", lambda e, q4=q4, r4=r4, sbb=sbb: e.tensor_tensor(r4[:, 1], q4[:, :, :, 8:16], sbb, ALU.mult), r=kQN + ["sin_t"], w=kR4)
                        A("dve", lambda e, q4=q4, r4=r4, cb=cb: e.tensor_tensor(r4[:, 2], q4[:, :, :, 8:16], cb, ALU.mult), r=kQN + ["cos_t"], w=kR4)
                        A("dve", lambda e, q4=q4, r4=r4, sbb=sbb: e.tensor_tensor(r4[:, 3], q4[:, :, :, 0:8], sbb, ALU.mult), r=kQN + ["sin_t"], w=kR4)
                        A("dve", lambda e, o4=o4, r4=r4: e.tensor_tensor(o4[:, :, :, 0:8], r4[:, 0], r4[:, 1], ALU.subtract), r=kR4 + [kt], w=[kt])
                        A("dve", lambda e, o4=o4, r4=r4: e.tensor_tensor(o4[:, :, :, 8:16], r4[:, 2], r4[:, 3], ALU.add), r=kR4 + [kt], w=[kt])

                def emit_T(tg):
                    s4, bQ, bV, bT, tiles, tb, kt = b2_geom(tg)
                    for tl in range(4):
                        for a in range(2):
                            A("pe", lambda e, a=a, tl=tl, tb=tb, bT=bT: e.transpose(pbb(bT)[:, a * 512 + tl * 128:a * 512 + (tl + 1) * 128], tb[:, tl * 256 + a * 128:tl * 256 + (a + 1) * 128], ident_b[:]),
                              r=[kt, "ident_b"], w=[("pb", bT)])
                    A("act", lambda e, tg=tg, bT=bT: e.copy(QK[:, :, tg * 512:(tg + 1) * 512], pbb(bT).rearrange("p (a c) -> p a c", a=2)),
                      r=[("pb", bT)], w=[("QK", t) for t in tiles])
                    pv = pb[bV].rearrange("p (t c) -> p t c", t=4)
                    if is_fox:
                        A("act", lambda e, tg=tg, pv=pv: e.copy(Vu[:, 4 * tg:4 * tg + 4, 0:64], pv[:, :, 0:64]), r=[("pb", bV)], w=[("Vu", t) for t in tiles])
                        A("act", lambda e, tg=tg, pv=pv: e.copy(Vu[:, 4 * tg:4 * tg + 4, 192:256], pv[:, :, 64:128]), r=[("pb", bV)], w=[("Vu", t) for t in tiles])
                    else:
                        A("act", lambda e, tg=tg, pv=pv: e.copy(Vu[:, 4 * tg:4 * tg + 4, 0:128], pv), r=[("pb", bV)], w=[("Vu", t) for t in tiles])


                emit_Mqk(0)
                emit_Mv(0)
                emit_CH(0)
                emit_Mqk(1)
                emit_Mv(1)
                emit_CH(1)
                emit_Mqk(2)
                emit_T(0)
                emit_Mv(2)
                emit_CH(2)
                emit_Mqk(3)
                emit_T(1)
                emit_Mv(3)
                emit_CH(3)
                emit_T(2)
                emit_T(3)

                steps = [(q, kb) for q in range(NQ) for kb in range(4 * (q + 1))]

                def geom(q, kb):
                    jd = kb - 4 * q
                    c0 = max(0, jd) * 128
                    return jd, c0, 512 - c0

                def emit_S(q, kb):
                    jd, c0, N = geom(q, kb)
                    sbk = [(kb % 2) * 2, (kb % 2) * 2 + 1]
                    qk_keys = [("QK", t) for t in range(4 * q, 4 * q + 4)]
                    for a in range(2):
                        pa = slice(a * 64, (a + 1) * 64)
                        A("pe", lambda e, a=a, pa=pa, kb=kb, c0=c0, N=N, q=q, sbk=sbk: e.matmul(pb[sbk[a]][:, 0:N], QK[pa, 1, tsl(kb)], QK[pa, 0, q * 512 + c0:(q + 1) * 512], start=True, stop=True),
                          r=[("QK", kb)] + qk_keys, w=[("pb", sbk[a])])

                def emit_E(q, kb):
                    jd, c0, N = geom(q, kb)
                    sbk = [(kb % 2) * 2, (kb % 2) * 2 + 1]
                    if not is_fox:
                        sb0 = sbk[0]
                        pi0 = next_pr()
                        next_pr()
                        A("act", lambda e, pi0=pi0, sb0=sb0, N=N: e.activation(PR[:, pi0:pi0 + 2, 0:N], PSa[:, sb0:sb0 + 2, 0:N], AF.Exp, scale=0.125),
                          r=[("pb", sb0), ("pb", sb0 + 1)], w=[("PR", pi0), ("PR", pi0 + 1)])
                        if jd >= 0:
                            A("dve", lambda e, pi0=pi0: e.tensor_tensor(PR[:, pi0:pi0 + 2, 0:128], PR[:, pi0:pi0 + 2, 0:128],
                                                                      tri_b[:].unsqueeze(1).to_broadcast([128, 2, 128]), ALU.mult),
                              r=[("PR", pi0), ("PR", pi0 + 1), "tri_b"], w=[("PR", pi0), ("PR", pi0 + 1)])
                        return [pi0, pi0 + 1]
                    prs = []
                    for a in range(2):
                        pi = next_pr()
                        prs.append(pi)
                        if not is_fox:
                            A("act", lambda e, a=a, pi=pi, N=N, sbk=sbk: e.activation(PR[:, pi, 0:N], pb[sbk[a]][:, 0:N], AF.Exp, scale=0.125),
                              r=[("pb", sbk[a])], w=[("PR", pi)])
                        else:
                            for s2 in range(2):
                                lo = max(c0, s2 * 256)
                                hi = (s2 + 1) * 256
                                if lo >= hi:
                                    continue
                                qb2 = 2 * q + s2
                                A("act", lambda e, a=a, pi=pi, lo=lo, hi=hi, c0=c0, kb=kb, qb2=qb2, sbk=sbk: e.activation(PR[:, pi, lo - c0:hi - c0], pb[sbk[a]][:, lo - c0:hi - c0], AF.Exp,
                                                                                                           bias=biasu[:, a, kb, qb2:qb2 + 1], scale=0.125),
                                  r=[("pb", sbk[a]), "biasu"], w=[("PR", pi)])
                        if jd >= 0:
                            A("dve", lambda e, pi=pi: e.tensor_tensor(PR[:, pi, 0:128], PR[:, pi, 0:128], tri_b[:], ALU.mult), r=[("PR", pi), "tri_b"], w=[("PR", pi)])
                    return prs

                def emit_AV(q, kb, prs):
                    jd, c0, N = geom(q, kb)
                    nkb = 4 * (q + 1)
                    if not is_fox:
                        for a in range(2):
                            ob = 4 + a
                            lb = 6 + a
                            pi = prs[a]
                            A("pe", lambda e, ob=ob, pi=pi, kb=kb, c0=c0, N=N, nkb=nkb: e.matmul(pb[ob][:, c0:512], Vu[:, kb, 0:128], PR[:, pi, 0:N], start=(kb == 0), stop=(kb == nkb - 1)),
                              r=[("PR", pi), ("Vu", kb)], w=[("pb", ob)])
                            A("pe", lambda e, lb=lb, pi=pi, kb=kb, c0=c0, N=N, nkb=nkb: e.matmul(pb[lb][:, c0:512], ones_b[:], PR[:, pi, 0:N], start=(kb == 0), stop=(kb == nkb - 1)),
                              r=[("PR", pi), "ones_b"], w=[("pb", lb)])
                    else:
                        for a in range(2):
                            ob = 4 + 2 * (q % 2) + a
                            pi = prs[a]
                            A("pe", lambda e, a=a, ob=ob, pi=pi, kb=kb, c0=c0, N=N, nkb=nkb: e.matmul(pb[ob][:, c0:512], Vu[:, kb, a * 128:(a + 1) * 128], PR[:, pi, 0:N], start=(kb == 0), stop=(kb == nkb - 1)),
                              r=[("PR", pi), ("Vu", kb)], w=[("pb", ob)])

                def emit_post_a(q):
                    if not is_fox:
                        O1, O2, L1, L2 = 4, 5, 6, 7
                        A("act", lambda e: e.activation(TF[2], pb[L1], AF.Ln), r=[("pb", L1)], w=["TF2"])
                        A("act", lambda e: e.activation(TF[3], pb[L2], AF.Ln), r=[("pb", L2)], w=["TF3"])
                        A("dve", lambda e: e.tensor_copy(TF[0], pb[O1]), r=[("pb", O1)], w=["TF0"])
                        A("dve", lambda e: e.tensor_copy(TF[1], pb[O2]), r=[("pb", O2)], w=["TF1"])
                    else:
                        OA = 4 + 2 * (q % 2)
                        OB = 5 + 2 * (q % 2)
                        tf = TF[q % 2]
                        tk = "TF%d" % (q % 2)
                        A("dve", lambda e, OA=OA, tf=tf: e.reciprocal(tf[0:64, :], pb[OA][64:128, :]), r=[("pb", OA)], w=[tk])
                        A("dve", lambda e, OB=OB, tf=tf: e.reciprocal(tf[64:128, :], pb[OB][0:64, :]), r=[("pb", OB)], w=[tk])
                        A("dve", lambda e, OA=OA, q=q, tf=tf, j=j: e.tensor_tensor(yT[0:64, 8 + j, qsl(q)], pb[OA][0:64, :], tf[0:64, :], ALU.mult),
                          r=[("pb", OA), tk], w=[("yT", 8 + j, q)])
                        A("dve", lambda e, OB=OB, q=q, tf=tf, j=j: e.tensor_tensor(yT[64:128, 8 + j, qsl(q)], pb[OB][64:128, :], tf[64:128, :], ALU.mult),
                          r=[("pb", OB), tk], w=[("yTb", 8 + j, q)])

                def emit_post_b(q, bk):
                    if is_fox:
                        return
                    A("act", lambda e: e.activation(TF[2], TF[2], AF.Exp, scale=-1.0), r=["TF2"], w=["TF2"])
                    A("act", lambda e: e.activation(TF[3], TF[3], AF.Exp, scale=-1.0), r=["TF3"], w=["TF3"])
                    A("dve", lambda e: e.tensor_tensor(TF[0], TF[0], TF[2], ALU.mult), r=["TF0", "TF2"], w=["TF0"])
                    A("dve", lambda e: e.tensor_tensor(TF[1], TF[1], TF[3], ALU.mult), r=["TF1", "TF3"], w=["TF1"])
                    A("dve", lambda e: e.scalar_tensor_tensor(TF[0], TF[1], lam4[:, 5:6], TF[0], ALU.mult, ALU.add), r=["TF0", "TF1", "lam4"], w=["TF0"])
                    A("act", lambda e: e.activation(TB[0][:, 0:512], TF[0], AF.Square), r=["TF0"], w=[("TB", 0)])
                    A("pe", lambda e, bk=bk: e.matmul(pb[bk], ones_b[:], TB[0][:, 0:512], start=True, stop=True), r=[("TB", 0), "ones_b"], w=[("pb", bk)])
                    A("act", lambda e, bk=bk: e.activation(TF[1], pb[bk], AF.Ln, bias=eps_t[:], scale=1.0 / 128), r=[("pb", bk), "eps_t"], w=["TF1"])
                    A("act", lambda e: e.activation(TF[1], TF[1], AF.Exp, scale=-0.5), r=["TF1"], w=["TF1"])
                    A("dve", lambda e, q=q, j=j: e.scalar_tensor_tensor(yT[:, 4 + j, qsl(q)], TF[0], subg[:, 0:1], TF[1], ALU.mult, ALU.mult),
                      r=["TF0", "TF1", "subg"], w=[("yT", 4 + j, q)])

                emit_S(*steps[0])
                pend_a = None
                pend_b = None
                for i, (q, kb) in enumerate(steps):
                    if i + 1 < len(steps):
                        emit_S(*steps[i + 1])
                    prs = emit_E(q, kb)
                    if pend_a is not None:
                        emit_post_a(pend_a)
                        pend_b = (pend_a, i + 2)
                        pend_a = None
                    emit_AV(q, kb, prs)
                    if pend_b is not None and i >= pend_b[1]:
                        emit_post_b(pend_b[0], (kb % 2) * 2 + 1)
                        pend_b = None
                    if kb == 4 * (q + 1) - 1:
                        pend_a = q
                if pend_b is not None:
                    emit_post_b(pend_b[0], 0)
                emit_post_a(pend_a)
                emit_post_b(pend_a, 0)

            fence(K_UNIT, K_M4)
            fence(K_PR, ["WO"])
            M4 = R16[:].rearrange("p (c s) -> p c s", c=4)
            WO = PR[:].rearrange("p a c -> p (a c)").rearrange("p (c d) -> p c d", c=4)
            yT_keys = lambda n, k, q: [("yT", n * 4 + k, q)] + ([("yTb", n * 4 + k, q)] if n == 2 else [])
            for half in range(2):
                G.dma("pool", WO, w_out_d[li].rearrange("(k p) d -> p k d", p=128)[:, 4 * half:4 * half + 4, :], w=["WO"], slot="WO")
                for dc in range(4):
                    dch = 4 * half + dc
                    for n in range(3):
                        si = next_slot()
                        wk = ("wsl", si)
                        wv = wsl[si][:, 0:1536].rearrange("p (k c) -> p k c", k=12)
                        G.dma("pool", wv[:, 0:8, :], w_in_v[:, :, O_GZ + n * D + dch * 128:O_GZ + n * D + (dch + 1) * 128], w=[wk], slot=wk, nfill=2)
                        G.dma("pool", wv[:, 8:12, :], w_br_d[li, n].rearrange("(k p) d -> p k d", p=128)[:, :, dch * 128:(dch + 1) * 128], w=[wk], slot=wk, cont=True)
                        for q in range(NQ):
                            bg = 2 * (q % 2)
                            br_ = 2 * (q % 2) + 1
                            for k in range(8):
                                A("pe", lambda e, k=k, q=q, bg=bg, wv=wv: e.matmul(pb[bg], wv[:, k, :], hT[:, k, qsl(q)], start=(k == 0), stop=(k == 7)),
                                  r=[wk] + hT_all[4 * q:4 * q + 4], w=[("pb", bg)])
                            for k in range(4):
                                A("pe", lambda e, k=k, q=q, br_=br_, n=n, wv=wv: e.matmul(pb[br_], wv[:, 8 + k, :], yT[:, n * 4 + k, qsl(q)], start=(k == 0), stop=(k == 3)),
                                  r=[wk] + yT_keys(n, k, q), w=[("pb", br_)])
                            sg = TB[q % 2]
                            sgk = ("TB", q % 2)
                            A("act", lambda e, sg=sg, bg=bg: e.activation(sg[:, 0:512], pb[bg], AF.Sigmoid), r=[("pb", bg)], w=[sgk])
                            mk = "TF%d" % q
                            if n == 0:
                                A("dve", lambda e, q=q, sg=sg, br_=br_: e.tensor_tensor(TF[q], sg[:, 0:512], pb[br_], ALU.mult), r=[sgk, ("pb", br_)], w=[mk])
                            else:
                                A("dve", lambda e, sg=sg, br_=br_: e.tensor_tensor(sg[:, 512:1024], sg[:, 0:512], pb[br_], ALU.mult), r=[sgk, ("pb", br_)], w=[sgk])
                                if n == 1:
                                    A("dve", lambda e, q=q, sg=sg: e.tensor_tensor(TF[q], TF[q], sg[:, 512:1024], ALU.add), r=[sgk, mk], w=[mk])
                                else:
                                    A("dve", lambda e, q=q, sg=sg, dc=dc: e.tensor_tensor(M4[:, dc, qsl(q)], TF[q], sg[:, 512:1024], ALU.add), r=[sgk, mk], w=[("M4", dc, q)])
                for t in range(NT):
                    for cg in range(2):
                        bank = 4 + (t * 2 + cg) % 4
                        for dc in range(4):
                            A("pe", lambda e, t=t, cg=cg, dc=dc, bank=bank: e.matmul(pb[bank], M4[:, dc, tsl(t)], WO[:, dc, cg * 512:(cg + 1) * 512], start=(dc == 0), stop=(dc == 3)),
                              r=[("M4", dc, t // 4), "WO"], w=[("pb", bank)])
                        A("dve", lambda e, t=t, cg=cg, bank=bank: e.tensor_tensor(x_sb[:, t, cg * 512:(cg + 1) * 512], x_sb[:, t, cg * 512:(cg + 1) * 512], pb[bank], ALU.add),
                          r=[("pb", bank), ("x", t)], w=[("x", t)])

            rmsnorm_to_hT(ffn_g_d[li])
            G.dma("pool", w_r[:, :, 0:4], wrg_d[li].rearrange("(k p) c -> p k c", p=128), w=["w_r"], slot="w_r", nfill=2)
            G.dma("pool", w_r[:, :, 4:20], wre_d[li].rearrange("(k p) c -> p k c", p=128), w=["w_r"], slot="w_r", cont=True)
            G.dma("sp", b_r[:, 0:4], brg_d[li].partition_broadcast(128), w=["b_r"], slot="b_r", nfill=2)
            G.dma("sp", b_r[:, 4:20], bre_d[li].partition_broadcast(128), w=["b_r"], slot="b_r", cont=True)
            for t in range(NT):
                for k in range(8):
                    A("pe", lambda e, t=t, k=k: e.matmul(pb[0][:, t * 20:(t + 1) * 20], hT[:, k, tsl(t)], w_r[:, k, :], start=(k == 0), stop=(k == 7)),
                      r=[("hT", t), "w_r"], w=[("pb", 0)])
            fence(["TF2", "TF3"], ["LE", "RA", "RB", "comb"])
            K_RS = ["LG", "RP", "gmax", "gsum", "m1", "m2", "wsum", "coef"]
            fence([("TB", 1)], K_RS)
            pl = pb[0][:, 0:320].rearrange("p (t c) -> p t c", c=20)
            A("dve", lambda e: e.tensor_tensor(LG, pl[:, :, 0:4], b_r[:, 0:4].unsqueeze(1).to_broadcast([128, NT, 4]), ALU.add), r=[("pb", 0), "b_r"], w=["LG"])
            A("dve", lambda e: e.tensor_tensor(LE, pl[:, :, 4:20], b_r[:, 4:20].unsqueeze(1).to_broadcast([128, NT, 16]), ALU.add), r=[("pb", 0), "b_r"], w=["LE"])
            bc4 = lambda ap: ap.unsqueeze(2).to_broadcast([128, NT, 4])
            bc16 = lambda ap: ap.unsqueeze(2).to_broadcast([128, NT, 16])
            gmax, gsum, m1, m2, wsum, coef = (RS[:, i, :] for i in range(6))
            A("dve", lambda e: e.tensor_reduce(gmax, LG, AX.X, ALU.max), r=["LG"], w=["gmax"])
            A("dve", lambda e: e.tensor_tensor(RA[:, :, 0:4], LG, bc4(gmax), ALU.subtract), r=["LG", "gmax"], w=["RA"])
            A("act", lambda e: e.activation(RA[:, :, 0:4], RA[:, :, 0:4], AF.Exp), r=["RA"], w=["RA"])
            A("dve", lambda e: e.tensor_reduce(gsum, RA[:, :, 0:4], AX.X, ALU.add), r=["RA"], w=["gsum"])
            A("dve", lambda e: e.tensor_tensor(RP, LG, bc4(gmax), ALU.is_ge), r=["LG", "gmax"], w=["RP"])
            A("dve", lambda e: e.tensor_scalar(RP, RP, 1.0, BIG, ALU.subtract, ALU.mult), r=["RP"], w=["RP"])
            A("dve", lambda e: e.tensor_tensor(LE.rearrange("p t (g x) -> p (t g) x", g=4), LE.rearrange("p t (g x) -> p (t g) x", g=4),
                                               RP.rearrange("p t g -> p (t g)").unsqueeze(2).to_broadcast([128, NT * 4, 4]), ALU.add),
              r=["LE", "RP"], w=["LE"])
            A("dve", lambda e: e.tensor_reduce(m1, LE, AX.X, ALU.max), r=["LE"], w=["m1"])
            A("dve", lambda e: e.tensor_tensor(RB, LE, bc16(m1), ALU.is_ge), r=["LE", "m1"], w=["RB"])
            A("dve", lambda e: e.scalar_tensor_tensor(RB, RB, -BIG, LE, ALU.mult, ALU.add), r=["RB", "LE"], w=["RB"])
            A("dve", lambda e: e.tensor_reduce(m2, RB, AX.X, ALU.max), r=["RB"], w=["m2"])
            A("dve", lambda e: e.tensor_tensor(RB, LE, bc16(m2), ALU.is_ge), r=["LE", "m2"], w=["RB"])
            A("dve", lambda e: e.tensor_tensor(RA, LE, bc16(m1), ALU.subtract), r=["LE", "m1"], w=["RA"])
            A("act", lambda e: e.activation(RA, RA, AF.Exp), r=["RA"], w=["RA"])
            A("dve", lambda e: e.tensor_tensor(RA, RA, RB, ALU.mult), r=["RA", "RB"], w=["RA"])
            A("dve", lambda e: e.tensor_reduce(wsum, RA, AX.X, ALU.add), r=["RA"], w=["wsum"])
            A("dve", lambda e: e.tensor_tensor(coef, wsum, gsum, ALU.mult), r=["wsum", "gsum"], w=["coef"])
            A("dve", lambda e: e.reciprocal(coef, coef), r=["coef"], w=["coef"])
            A("dve", lambda e: e.tensor_tensor(comb, RA, bc16(coef), ALU.mult), r=["RA", "coef"], w=["comb"])
            fence(K_M4, K_MOE16)
            fence(K_YT, K_MOEY)
            combT = R16f[0:16, 0:2048]
            selE = R16f[0:16, 2048:4096].rearrange("p (e m) -> p e m", e=16)
            A("pool", lambda e: e.memset(R16f[0:16, 2048:4096], 1.0), w=["selE"])
            A("pool", lambda e: e.affine_select(R16f[0:16, 2048:4096], R16f[0:16, 2048:4096], [[-1, 16], [0, 128]], ALU.is_equal, 0.0, base=0, channel_multiplier=1),
              r=["selE"], w=["selE"])
            for t in range(NT):
                bank = 1 + t // 4
                A("pe", lambda e, t=t, bank=bank: e.transpose(pb[bank][0:16, (t % 4) * 128:(t % 4 + 1) * 128], comb[:, t, :], ident_f[:]),
                  r=["comb", "ident_f"], w=[("pb", bank)])
            for i in range(4):
                A("dve", lambda e, i=i: e.tensor_copy(combT[:, qsl(i)], pb[1 + i][0:16, :]), r=[("pb", 1 + i)], w=["combT"])
            fence(["LE", "RA", "RB", "comb"], ["TF2", "TF3"])
            fence(K_RS, [("TB", 1)])
            G.dma("sp", comb_scr, combT, r=["combT"], w=["comb_scr"], slot="comb_scr")
            yTf = yT[:].rearrange("p a s -> p (a s)")
            ACT4 = yTf[:, 0:16384].rearrange("p (c s) -> p c s", c=8)
            WD = yTf[:, 16384:24576].rearrange("p (e f d) -> p e f d", e=4, f=2)
            for g4 in range(4):
                for e4 in range(4):
                    ex = 4 * g4 + e4
                    si = next_slot()
                    wk = ("wsl", si)
                    wv = wsl[si][:].rearrange("p (k c) -> p k c", k=8)
                    G.dma("pool", wv[:, :, 0:256], mg_d[li, ex].rearrange("(k p) f -> p k f", p=128), w=[wk], slot=wk, nfill=2)
                    G.dma("pool", wv[:, :, 256:512], mu_d[li, ex].rearrange("(k p) f -> p k f", p=128), w=[wk], slot=wk, cont=True)
                    G.dma("pool", WD[:, e4], md_d[li, ex].rearrange("(f p) d -> p f d", p=128), w=[("WD", e4)], slot=("WD", e4))
                    for q in range(NQ):
                        cs = 2 + (ex * NQ + q) % 2
                        ck = "TF%d" % cs
                        G.dma("sp", TF[cs], comb_scr[ex, q * 512:(q + 1) * 512].partition_broadcast(128), r=["comb_scr"], w=[ck], slot=("cb", cs))
                        for fc in range(2):
                            bg = 2 * fc
                            bu = 2 * fc + 1
                            for k in range(8):
                                A("pe", lambda e, k=k, q=q, fc=fc, bg=bg, wv=wv: e.matmul(pb[bg], wv[:, k, fc * 128:(fc + 1) * 128], hT[:, k, qsl(q)], start=(k == 0), stop=(k == 7)),
                                  r=[wk] + hT_all[4 * q:4 * q + 4], w=[("pb", bg)])
                            for k in range(8):
                                A("pe", lambda e, k=k, q=q, fc=fc, bu=bu, wv=wv: e.matmul(pb[bu], wv[:, k, 256 + fc * 128:256 + (fc + 1) * 128], hT[:, k, qsl(q)], start=(k == 0), stop=(k == 7)),
                                  r=[wk] + hT_all[4 * q:4 * q + 4], w=[("pb", bu)])
                            A("act", lambda e, fc=fc, bg=bg: e.activation(TF[fc], pb[bg], AF.Silu), r=[("pb", bg)], w=["TF%d" % fc])
                            A("dve", lambda e, fc=fc, bu=bu: e.tensor_tensor(TF[fc], TF[fc], pb[bu], ALU.mult), r=["TF%d" % fc, ("pb", bu)], w=["TF%d" % fc])
                            A("dve", lambda e, fc=fc, e4=e4, q=q, cs=cs: e.tensor_tensor(ACT4[:, e4 * 2 + fc, qsl(q)], TF[fc], TF[cs], ALU.mult),
                              r=["TF%d" % fc, ck], w=[("ACT4", e4 * 2 + fc, q)])
                for t in range(NT):
                    for cg in range(2):
                        bank = 4 + (t * 2 + cg) % 3
                        for c in range(8):
                            A("pe", lambda e, t=t, cg=cg, c=c, bank=bank: e.matmul(pb[bank], ACT4[:, c, tsl(t)], WD[:, c // 2, c % 2, cg * 512:(cg + 1) * 512], start=(c == 0), stop=(c == 7)),
                              r=[("ACT4", c, t // 4), ("WD", c // 2)], w=[("pb", bank)])
                        A("dve", lambda e, t=t, cg=cg, bank=bank: e.tensor_tensor(x_sb[:, t, cg * 512:(cg + 1) * 512], x_sb[:, t, cg * 512:(cg + 1) * 512], pb[bank], ALU.add),
                          r=[("pb", bank), ("x", t)], w=[("x", t)])

            rmsnorm_to_hT(ple_g_d[li])
            PB = R16[:, 0:4096].rearrange("p (t c) -> p t c", t=NT)
            PT = R16[:, 4096:8192].rearrange("p (k s) -> p k s", k=2)
            WP = PR[:].rearrange("p a c -> p (a c)")[:, 0:2048].rearrange("p (k d) -> p k d", k=2)
            fence(K_MOE16, K_PLE16)
            fence(["WO"], ["WP"])
            G.dma("pool", PB, p_d[li].rearrange("(t p) c -> p t c", p=128), w=["PB"], slot="PB")
            G.dma("pool", WP, wp_d[li].rearrange("(k p) d -> p k d", p=128), w=["WP"], slot="WP")
            for t in range(NT):
                bank = 2 + t % 2
                for k in range(2):
                    A("pe", lambda e, t=t, k=k, bank=bank: e.transpose(pbb(bank)[:, k * 128:(k + 1) * 128], PB[:, t, k * 128:(k + 1) * 128], ident_b[:]),
                      r=["PB", "ident_b"], w=[("pb", bank)])
                A("act", lambda e, t=t, bank=bank: e.copy(PT[:, :, tsl(t)], pbb(bank)[:, 0:256].rearrange("p (k c) -> p k c", k=2)),
                  r=[("pb", bank)], w=[("PT", t)])
            for cg in range(2):
                si = next_slot()
                wk = ("wsl", si)
                wv = wsl[si][:].rearrange("p (k c) -> p k c", k=8)
                G.dma("pool", wv, wpg_d[li].rearrange("(k p) c -> p k c", p=128)[:, :, cg * 512:(cg + 1) * 512], w=[wk], slot=wk)
                for t in range(NT):
                    ba = 4 + 2 * (t % 2)
                    bb_ = 5 + 2 * (t % 2)
                    for k in range(8):
                        A("pe", lambda e, t=t, k=k, ba=ba, wv=wv: e.matmul(pb[ba], hT[:, k, tsl(t)], wv[:, k, :], start=(k == 0), stop=(k == 7)),
                          r=[("hT", t), wk], w=[("pb", ba)])
                    for k in range(2):
                        A("pe", lambda e, t=t, k=k, bb_=bb_, cg=cg: e.matmul(pb[bb_], PT[:, k, tsl(t)], WP[:, k, cg * 512:(cg + 1) * 512], start=(k == 0), stop=(k == 1)),
                          r=[("PT", t), "WP"], w=[("pb", bb_)])
                    tf = TF[t % 2]
                    tk = "TF%d" % (t % 2)
                    A("act", lambda e, tf=tf, ba=ba: e.activation(tf[:], pb[ba], AF.Sigmoid), r=[("pb", ba)], w=[tk])
                    A("dve", lambda e, tf=tf, bb_=bb_: e.tensor_tensor(tf[:], tf[:], pb[bb_], ALU.mult), r=[tk, ("pb", bb_)], w=[tk])
                    A("dve", lambda e, t=t, cg=cg, tf=tf: e.tensor_tensor(x_sb[:, t, cg * 512:(cg + 1) * 512], x_sb[:, t, cg * 512:(cg + 1) * 512], tf[:], ALU.add),
                      r=[tk, ("x", t)], w=[("x", t)])

        ov = out_d.rearrange("(t p) d -> p t d", p=128)
        for i, (t0, t1) in enumerate([(0, 4), (4, 8), (8, 12), (12, 14), (14, 15), (15, 16)]):
            G.dma("sp", ov[:, t0:t1, :], x_sb[:, t0:t1, :], r=[("x", t) for t in range(t0, t1)], slot=("xs", i))
        with nc.allow_non_contiguous_dma(reason="tiny per-layer parameter vectors"):
            G.emit()
    return nc


W_NAMES = ["attn_norm_g", "w_in", "pool_w", "pool_scale", "diff_qn_g", "diff_kn_g", "diff_lambda", "diff_subln_g",
           "fox_qn_g", "fox_kn_g", "fox_forget_b", "w_branch", "w_out", "ffn_norm_g", "w_route_group", "b_route_group",
           "w_route_expert", "b_route_expert", "moe_w_gate", "moe_w_up", "moe_w_down", "ple_norm_g", "w_ple_gate", "w_ple"]

_CACHE = {}
FUSED = True


def _prog(NL, layer0):
    key = (NL, layer0)
    if key not in _CACHE:
        _CACHE[key] = build_program(NL, layer0)
    return _CACHE[key]


def _run(xin, inputs, layers):
    NL = len(layers)
    l0 = layers[0]
    nc = _prog(NL, l0)
    shared = {}
    for n in W_NAMES:
        a = np.asarray(inputs[n], dtype=np.float32)[l0:l0 + NL]
        if n == "diff_lambda":
            a = a.reshape(NL, 256)
        shared[n] = np.ascontiguousarray(a)
    p = np.asarray(inputs["p"], dtype=np.float32)
    pos = np.asarray(inputs["positions"], dtype=np.int32)
    in_maps = []
    for b in range(8):
        m = dict(shared)
        m["x"] = np.ascontiguousarray(xin[b])
        m["p"] = np.ascontiguousarray(p[l0:l0 + NL, b])
        m["positions"] = np.ascontiguousarray(pos[b].reshape(NT, 128))
        in_maps.append(m)
    res = run_bass_kernel_spmd(nc, in_maps, core_ids=list(range(8)))
    return np.stack([np.asarray(r["out"], dtype=np.float32) for r in res.results], axis=0)


def kernel(**inputs):
    x = np.asarray(inputs["x"], dtype=np.float32)
    if FUSED:
        return _run(x, inputs, [0, 1])
    x = _run(x, inputs, [0])
    x = _run(x, inputs, [1])
    return x
```

```python
import contextlib
import math
import numpy as np
import concourse.bass as bass
import concourse.mybir as mybir
from concourse.bass_utils import run_bass_kernel_spmd

F32 = mybir.dt.float32
BF16 = mybir.dt.bfloat16
I32 = mybir.dt.int32
AF = mybir.ActivationFunctionType
ALU = mybir.AluOpType
AX = mybir.AxisListType
ENGS = ("pe", "act", "dve", "pool", "sp")

S = 2048
D = 1024
NT = 16
NQ = 4
INW = 6664
EPS = 1e-6
BIG = 30000.0
O_POOL, O_DQ, O_DK, O_DV, O_FQ, O_FK, O_FV, O_FZ, O_GZ = 0, 512, 1024, 1536, 2048, 2560, 3072, 3584, 3592


class Graph:
    SAME_ENG_WINDOW = 10 ** 9

    def __init__(self, nc):
        self.nc = nc
        self.ops = []
        self.last_w = {}
        self.readers = {}
        self.stream = {e: [] for e in ENGS}
        self.dma_count = {}

    def _add(self, op, r, w):
        deps = set()
        for k in r:
            if k in self.last_w:
                deps.add(self.last_w[k])
        for k in w:
            if k in self.last_w:
                deps.add(self.last_w[k])
            for x in self.readers.get(k, ()):
                deps.add(x)
        idx = len(self.ops)
        op["deps"] = deps
        op["idx"] = idx
        op["signal"] = False
        self.ops.append(op)
        op["pos"] = len(self.stream[op["eng"]])
        self.stream[op["eng"]].append(idx)
        for k in w:
            self.last_w[k] = idx
            self.readers[k] = []
        for k in r:
            if k not in w:
                self.readers.setdefault(k, []).append(idx)
        return idx

    def add(self, eng, fn, r=(), w=()):
        return self._add({"eng": eng, "fn": fn, "kind": "c"}, tuple(r), tuple(w))

    def dma(self, eng, out, in_, r=(), w=(), slot=None, nfill=1, cont=False):
        if not cont:
            self.dma_count[slot] = self.dma_count.get(slot, 0) + nfill
        n = self.dma_count[slot]
        idx = self._add({"eng": eng, "kind": "d", "out": out, "in": in_, "slot": slot, "n": n},
                        tuple(r), tuple(w))
        op = self.ops[idx]
        op["deps"] = {d for d in op["deps"]
                      if not (self.ops[d]["kind"] == "d" and self.ops[d]["slot"] == slot and self.ops[d]["n"] == n)}
        return idx

    def emit(self):
        nc = self.nc
        ops = self.ops
        waits = [None] * len(ops)
        for e in ENGS:
            kn = {}
            for idx in self.stream[e]:
                op = ops[idx]
                need = {}
                for d in op["deps"]:
                    Dp = ops[d]
                    if Dp["kind"] == "d":
                        key = ("d", Dp["slot"])
                        val = Dp["n"]
                    else:
                        if Dp["eng"] == e:
                            if e == "pe" or op["pos"] - Dp["pos"] > self.SAME_ENG_WINDOW:
                                continue
                        key = ("e", Dp["eng"])
                        val = Dp["pos"]
                    if kn.get(key, -1) >= val:
                        continue
                    if need.get(key, (-1, None))[0] < val:
                        need[key] = (val, d)
                for key, (val, d) in need.items():
                    kn[key] = val
                    ops[d]["signal"] = True
                waits[idx] = [d for (_, d) in need.values()]
        cnt = {e: 0 for e in ENGS}
        for e in ENGS:
            for idx in self.stream[e]:
                op = ops[idx]
                if op["kind"] == "c" and op["signal"]:
                    cnt[e] += 1
                    op["val"] = cnt[e]
        self.counts = cnt
        with contextlib.ExitStack() as es:
            esem = {e: es.enter_context(nc.semaphore("c_" + e)) for e in ENGS}
            dsem = {}
            for s in self.dma_count:
                dsem[s] = es.enter_context(nc.semaphore("d_%d" % len(dsem)))
            block = es.enter_context(nc.Block())

            def run(e, eng):
                last = {}
                for idx in self.stream[e]:
                    op = ops[idx]
                    for d in waits[idx]:
                        Dp = ops[d]
                        if Dp["kind"] == "d":
                            eng.wait_ge(dsem[Dp["slot"]], 16 * Dp["n"])
                        else:
                            eng.wait_ge(esem[Dp["eng"]], Dp["val"])
                    if op["kind"] == "d":
                        eng.dma_start(out=op["out"], in_=op["in"]).then_inc(dsem[op["slot"]], 16)
                        last[op["slot"]] = max(last.get(op["slot"], 0), op["n"])
                    else:
                        ins = op["fn"](eng)
                        if op["signal"]:
                            ins.then_inc(esem[e], 1)
                for s, n in last.items():
                    eng.wait_ge(dsem[s], 16 * n)

            @block.tensor
            def _(eng):
                run("pe", eng)

            @block.scalar
            def _(eng):
                run("act", eng)

            @block.vector
            def _(eng):
                run("dve", eng)

            @block.gpsimd
            def _(eng):
                run("pool", eng)

            @block.sync
            def _(eng):
                run("sp", eng)


def build_program(NL, layer0):
    nc = bass.Bass("TRN2", target_bir_lowering=False)

    def din(name, shape, dt=F32):
        return nc.dram_tensor(name, list(shape), dt, kind="ExternalInput").ap()

    x_d = din("x", [S, D])
    p_d = din("p", [NL, S, 256])
    pos_d = din("positions", [NT, 128], I32)
    attn_g_d = din("attn_norm_g", [NL, D])
    w_in_d = din("w_in", [NL, D, INW])
    pool_w_d = din("pool_w", [NL, 4, 128, 128])
    pool_s_d = din("pool_scale", [NL, 512])
    dqg_d = din("diff_qn_g", [NL, 64])
    dkg_d = din("diff_kn_g", [NL, 64])
    dlam_d = din("diff_lambda", [NL, 256])
    dsub_d = din("diff_subln_g", [NL, 128])
    fqg_d = din("fox_qn_g", [NL, 64])
    fkg_d = din("fox_kn_g", [NL, 64])
    ffb_d = din("fox_forget_b", [NL, 8])
    w_br_d = din("w_branch", [NL, 3, 512, D])
    w_out_d = din("w_out", [NL, D, D])
    ffn_g_d = din("ffn_norm_g", [NL, D])
    wrg_d = din("w_route_group", [NL, D, 4])
    brg_d = din("b_route_group", [NL, 4])
    wre_d = din("w_route_expert", [NL, D, 16])
    bre_d = din("b_route_expert", [NL, 16])
    mg_d = din("moe_w_gate", [NL, 16, D, 256])
    mu_d = din("moe_w_up", [NL, 16, D, 256])
    md_d = din("moe_w_down", [NL, 16, 256, D])
    ple_g_d = din("ple_norm_g", [NL, D])
    wpg_d = din("w_ple_gate", [NL, D, D])
    wp_d = din("w_ple", [NL, 256, D])
    out_d = nc.dram_tensor("out", [S, D], F32, kind="ExternalOutput").ap()
    comb_scr = nc.dram_tensor("comb_scr", [16, S], F32, kind="Internal").ap()

    es = contextlib.ExitStack()
    with es:
        def sb(name, shape, dt):
            return es.enter_context(nc.sbuf_tensor(name, list(shape), dt))

        def ps(name, shape, dt):
            return es.enter_context(nc.psum_tensor(name, list(shape), dt))

        G = Graph(nc)
        _real_add = G.add
        defer = [False]
        deferred = []

        def A(eng, fn, r=(), w=()):
            if defer[0]:
                deferred.append((eng, fn, tuple(r), tuple(w)))
                return None
            return _real_add(eng, fn, r=r, w=w)

        def flush():
            while deferred:
                eng, fn, r, w = deferred.pop(0)
                _real_add(eng, fn, r=r, w=w)

        x_sb = sb("x_sb", [128, NT, D], F32)
        hT = sb("hT", [128, 8, S], BF16)
        wsl = [sb("wsl%d" % i, [128, 4096], BF16) for i in range(2)]
        yT = sb("yT", [128, 12, S], BF16)
        R16 = sb("R16", [128, 8192], BF16)
        PR = sb("PR", [128, 8, 512], BF16)
        TFa = sb("TFa", [128, 4, 512], F32)
        TF = [TFa[:, i, :] for i in range(4)]
        TB = [sb("TB%d" % i, [128, 1024], BF16) for i in range(2)]
        g_bc = TFa[:, 0:2, :].rearrange("p a c -> p (a c)")
        ident_f = sb("ident_f", [128, 128], F32)
        ones_f = sb("ones_f", [128, 128], F32)
        tri_f = sb("tri_f", [128, 128], F32)
        sel_last = sb("sel_last", [128, 128], F32)
        ident_b = sb("ident_b", [128, 128], BF16)
        ones_b = sb("ones_b", [128, 128], BF16)
        tri_b = sb("tri_b", [128, 128], BF16)
        eps_t = sb("eps_t", [128, 1], F32)
        one_t = sb("one_t", [128, 1], F32)
        pi_t = sb("pi_t", [128, 1], F32)
        zero_t = sb("zero_t", [128, 1], F32)
        cos_t = sb("cos_t", [128, NT, 8], F32)
        sin_t = sb("sin_t", [128, NT, 8], F32)
        sm = sb("sm", [128, 96], F32)
        qkg = [sb("qkg%d" % i, [128, 256], F32) for i in range(2)]
        ffb_bc = sb("ffb_bc", [128, 8], F32)
        Gc = sb("Gc", [128, NT, 8], F32)
        Gend = sb("Gend", [128, NT, 8], F32)
        zz = TFa[:, 2, 0:128].rearrange("p (t c) -> p t c", c=8)
        carry = TFa[:, 2, 128:256].rearrange("p (t c) -> p t c", c=8)
        biasu = sb("biasu", [128, 2, NT, NT], F32)
        wfz = sb("wfz", [128, 8, 8], BF16)
        poolw = sb("poolw", [128, 4, 128], BF16)
        pools = sb("pools", [128, 4], F32)
        ic16 = sb("ic16", [128, 4, 16], F32)
        lamt = TFa[:, 3, 0:256]
        lam4 = sb("lam4", [128, 8], F32)
        subg = sb("subg", [128, 1], F32)
        w_r = sb("w_r", [128, 8, 20], BF16)
        b_r = sb("b_r", [128, 20], F32)
        TB1f = TB[1][:].bitcast(F32)
        LG = TB1f[:, 0:64].rearrange("p (t c) -> p t c", c=4)
        LE = TFa[:, 2, 0:256].rearrange("p (t c) -> p t c", c=16)
        RA = TFa[:, 2, 256:512].rearrange("p (t c) -> p t c", c=16)
        RB = TFa[:, 3, 0:256].rearrange("p (t c) -> p t c", c=16)
        RS = TB1f[:, 128:256].rearrange("p (a t) -> p a t", a=8)
        RP = TB1f[:, 64:128].rearrange("p (t c) -> p t c", c=4)
        comb = TFa[:, 3, 256:512].rearrange("p (t c) -> p t c", c=16)
        halo = sb("halo", [128, 16], F32)
        PSa = ps("PSa", [128, 8, 512], F32)
        pb = [PSa[:, i, :] for i in range(8)]

        def pbb(i):
            return pb[i].bitcast(BF16)

        R16f = R16[:].bitcast(F32)
        tsl = lambda t: slice(t * 128, (t + 1) * 128)
        qsl = lambda q: slice(q * 512, (q + 1) * 512)

        fence_t = sb("fence_t", [128, 1], F32)

        def fence(old, new):
            A("dve", lambda e: e.memset(fence_t[:], 0.0), w=list(old) + list(new) + ["fence_t"])

        A("pool", lambda e: e.memset(ones_f[:], 1.0), w=["ones_f"])
        A("pool", lambda e: e.memset(ones_b[:], 1.0), w=["ones_b"])
        A("pool", lambda e: e.memset(eps_t[:], EPS), w=["eps_t"])
        A("pool", lambda e: e.memset(one_t[:], 1.0), w=["one_t"])
        A("pool", lambda e: e.memset(pi_t[:], math.pi), w=["pi_t"])
        A("pool", lambda e: e.memset(zero_t[:], 0.0), w=["zero_t"])
        A("pool", lambda e: e.affine_select(ident_f[:], ones_f[:], [[1, 128]], ALU.is_equal, 0.0, base=0, channel_multiplier=-1),
          r=["ones_f"], w=["ident_f"])
        A("pool", lambda e: e.affine_select(tri_f[:], ones_f[:], [[1, 128]], ALU.is_ge, 0.0, base=0, channel_multiplier=-1),
          r=["ones_f"], w=["tri_f"])
        A("pool", lambda e: e.affine_select(sel_last[:], ones_f[:], [[0, 128]], ALU.is_equal, 0.0, base=-127, channel_multiplier=1),
          r=["ones_f"], w=["sel_last"])
        A("pool", lambda e: e.tensor_copy(ident_b[:], ident_f[:]), r=["ident_f"], w=["ident_b"])
        A("pool", lambda e: e.tensor_copy(tri_b[:], tri_f[:]), r=["tri_f"], w=["tri_b"])
        A("pool", lambda e: e.iota(ic16[:], [[0, 4], [1, 16]], base=1, channel_multiplier=0, allow_small_or_imprecise_dtypes=True), w=["ic16"])
        for g in range(4):
            A("dve", lambda e, g=g: e.tensor_scalar(ic16[:, g, :], ic16[:, g, :], float(2 ** (g + 1)), None, ALU.min), r=["ic16"], w=["ic16"])
        A("dve", lambda e: e.reciprocal(ic16[:], ic16[:]), r=["ic16"], w=["ic16"])

        xv = x_d.rearrange("(t p) d -> p t d", p=128)
        for i, (t0, t1) in enumerate([(0, 1), (1, 2), (2, 4), (4, 8), (8, 12), (12, 16)]):
            G.dma("sp", x_sb[:, t0:t1, :], xv[:, t0:t1, :], w=[("x", t) for t in range(t0, t1)], slot=("xl", i))

        def build_rotary():
            fence([("TB", 0), ("TB", 1)], ["posi", "posf", "ang", "angi", "angr"])
            TB0f = TB[0][:].bitcast(F32)
            TB1i = TB[1][:].bitcast(I32)
            posi = TB1i[0:NT, 0:128]
            angi = TB1i[:, 128:256].rearrange("p (t c) -> p t c", c=8)
            posf = TB0f[0:NT, 0:128]
            ang = TB0f[:, 128:256].rearrange("p (t c) -> p t c", c=8)
            angr = TB0f[:, 256:384].rearrange("p (t c) -> p t c", c=8)
            post = sb("post", [128, NT], F32)
            invf = sb("invf", [128, 8], F32)
            G.dma("sp", posi, pos_d, w=["posi"], slot="posi")
            A("dve", lambda e: e.tensor_copy(posf, posi), r=["posi"], w=["posf"])
            A("pe", lambda e: e.transpose(pb[0][:, 0:NT], posf, ident_f[0:NT, 0:NT]), r=["posf", "ident_f"], w=[("pb", 0)])
            A("dve", lambda e: e.tensor_copy(post[:], pb[0][:, 0:NT]), r=[("pb", 0)], w=["post"])
            A("pool", lambda e: e.iota(invf[:], [[1, 8]], base=0, channel_multiplier=0, allow_small_or_imprecise_dtypes=True), w=["invf"])
            A("act", lambda e: e.activation(invf[:], invf[:], AF.Exp, scale=-math.log(500000.0) / 8.0), r=["invf"], w=["invf"])
            A("dve", lambda e: e.tensor_tensor(ang, post[:].unsqueeze(2).to_broadcast([128, NT, 8]),
                                               invf[:].unsqueeze(1).to_broadcast([128, NT, 8]), ALU.mult), r=["post", "invf"], w=["ang"])

            def sin_of(dst, shift, tag):
                A("dve", lambda e: e.tensor_scalar(angr, ang, shift, 1.0 / (2 * math.pi), ALU.add, ALU.mult), r=["ang"], w=["angr"])
                A("dve", lambda e: e.tensor_copy(angi, angr), r=["angr"], w=["angi"])
                A("dve", lambda e: e.tensor_copy(angr, angi), r=["angi"], w=["angr"])
                A("dve", lambda e: e.tensor_scalar(angr, angr, -2 * math.pi, shift, ALU.mult, ALU.add), r=["angr"], w=["angr"])
                A("dve", lambda e: e.tensor_tensor(angr, angr, ang, ALU.add), r=["angr", "ang"], w=["angr"])
                A("dve", lambda e: e.tensor_scalar(angr, angr, math.pi, -math.pi, ALU.min, ALU.max), r=["angr"], w=["angr"])
                A("act", lambda e: e.activation(dst[:], angr, AF.Sin), r=["angr"], w=[tag])

            sin_of(sin_t, 0.0, "sin_t")
            sin_of(cos_t, math.pi / 2, "cos_t")

            fence(["posi", "posf", "ang", "angi", "angr"], [("TB", 0), ("TB", 1)])


        sn = sb("sn", [128, 48], F32)

        def rmsnorm_to_hT(g_dram_row):
            fence(["TF0", "TF1", "TF2", "TF3"], ["g_bc", "nj0", "nj1"])
            G.dma("sp", g_bc, g_dram_row.partition_broadcast(128), w=["g_bc"], slot="g_bc")
            junk = [TFa[:, 2, :].bitcast(BF16), TFa[:, 3, :].bitcast(BF16)]

            def s1(t):
                jk = junk[t % 2]
                kj = "nj%d" % (t % 2)
                A("act", lambda e, t=t, jk=jk: e.activation(jk, x_sb[:, t, :], AF.Square, accum_out=sn[:, t:t + 1]), r=[("x", t)], w=[kj, ("sn", t)])
                A("act", lambda e, t=t: e.activation(sn[:, 16 + t:17 + t], sn[:, t:t + 1], AF.Ln, bias=eps_t[:], scale=1.0 / D), r=[("sn", t), "eps_t"], w=[("sn1", t)])
                A("act", lambda e, t=t: e.activation(sn[:, 32 + t:33 + t], sn[:, 16 + t:17 + t], AF.Exp, scale=-0.5), r=[("sn1", t)], w=[("sn2", t)])

            def s2a(t):
                tb = TB[t % 2]
                kt = ("TB", t % 2)
                bank = t % 2
                A("dve", lambda e, t=t, tb=tb: e.scalar_tensor_tensor(tb[:], x_sb[:, t, :], sn[:, 32 + t:33 + t], g_bc, ALU.mult, ALU.mult),
                  r=[("x", t), ("sn2", t), "g_bc"], w=[kt])
                for k in range(8):
                    A("pe", lambda e, k=k, tb=tb, bank=bank: e.transpose(pbb(bank)[:, k * 128:(k + 1) * 128], tb[:, k * 128:(k + 1) * 128], ident_b[:]),
                      r=[kt, "ident_b"], w=[("pb", bank)])

            def s2b(t):
                bank = t % 2
                A("dve", lambda e, t=t, bank=bank: e.tensor_copy(hT[:, :, tsl(t)], pbb(bank).rearrange("p (k c) -> p k c", k=8)),
                  r=[("pb", bank)], w=[("hT", t)])

            s1(0)
            s1(1)
            for t in range(NT):
                s2a(t)
                if t + 2 < NT:
                    s1(t + 2)
                if t >= 1:
                    s2b(t - 1)
            s2b(NT - 1)
            fence(["g_bc", "nj0", "nj1"], ["TF0", "TF1", "TF2", "TF3"])

        K_PR = [("PR", i) for i in range(8)]
        K_POOLST = ["U0_0", "U1_0", "U2_0", "DB_0", "U0_1", "U1_1", "U2_1", "DB_1"]
        K_UNIT = [("QK", t) for t in range(NT)] + [("Vu", t) for t in range(NT)]
        K_M4 = [("M4", dc, q) for dc in range(4) for q in range(NQ)]
        K_MOE16 = ["combT", "selE"]
        K_PLE16 = ["PB"] + [("PT", t) for t in range(NT)]
        K_YT = [("yT", c, q) for c in range(12) for q in range(NQ)] + [("yTb", c, q) for c in range(8, 12) for q in range(NQ)]
        K_MOEY = [("ACT4", c, q) for c in range(8) for q in range(NQ)] + [("WD", e4) for e4 in range(4)]

        slot_i = [0]

        def next_slot():
            i = slot_i[0] % 2
            slot_i[0] += 1
            return i

        for li in range(NL):
            lglob = layer0 + li
            lam_init = 0.8 - 0.6 * math.exp(-0.3 * lglob)
            w_in_v = w_in_d[li].rearrange("(k p) c -> p k c", p=128)

            rmsnorm_to_hT(attn_g_d[li])
            if li == 0:
                build_rotary()
            hT_all = [("hT", t) for t in range(NT)]

            G.dma("sp", qkg[0][:, 0:64], dqg_d[li].partition_broadcast(128), w=["qkg0"], slot="qkg0", nfill=4)
            G.dma("sp", qkg[0][:, 64:128], dqg_d[li].partition_broadcast(128), w=["qkg0"], slot="qkg0", cont=True)
            G.dma("sp", qkg[0][:, 128:192], dkg_d[li].partition_broadcast(128), w=["qkg0"], slot="qkg0", cont=True)
            G.dma("sp", qkg[0][:, 192:256], dkg_d[li].partition_broadcast(128), w=["qkg0"], slot="qkg0", cont=True)
            G.dma("sp", qkg[1][:, 0:64], fqg_d[li].partition_broadcast(128), w=["qkg1"], slot="qkg1", nfill=4)
            G.dma("sp", qkg[1][:, 64:128], fqg_d[li].partition_broadcast(128), w=["qkg1"], slot="qkg1", cont=True)
            G.dma("sp", qkg[1][:, 128:192], fkg_d[li].partition_broadcast(128), w=["qkg1"], slot="qkg1", cont=True)
            G.dma("sp", qkg[1][:, 192:256], fkg_d[li].partition_broadcast(128), w=["qkg1"], slot="qkg1", cont=True)
            G.dma("sp", ffb_bc[:], ffb_d[li].partition_broadcast(128), w=["ffb"], slot="ffb")
            fence(["TF3"], ["lamt"])
            G.dma("sp", lamt, dlam_d[li].partition_broadcast(128), w=["lamt"], slot="lamt")
            G.dma("sp", subg[:], dsub_d[li].rearrange("(p o) -> p o", o=1), w=["subg"], slot="subg")
            G.dma("sp", pools[:], pool_s_d[li].rearrange("(g p) -> p g", p=128), w=["pools"], slot="pools")
            G.dma("pool", poolw[:], pool_w_d[li].rearrange("g c d -> c g d"), w=["poolw"], slot="poolw")
            G.dma("pool", wfz[:], w_in_v[:, :, O_FZ:O_FZ + 8], w=["wfz"], slot="wfz")
            A("dve", lambda e: e.tensor_tensor(lamt[:, 0:64], lamt[:, 0:64], lamt[:, 64:128], ALU.mult), r=["lamt"], w=["lamt"])
            A("dve", lambda e: e.tensor_tensor(lamt[:, 128:192], lamt[:, 128:192], lamt[:, 192:256], ALU.mult), r=["lamt"], w=["lamt"])
            A("dve", lambda e: e.tensor_reduce(lam4[:, 0:1], lamt[:, 0:64], AX.X, ALU.add), r=["lamt"], w=["lam4"])
            A("dve", lambda e: e.tensor_reduce(lam4[:, 1:2], lamt[:, 128:192], AX.X, ALU.add), r=["lamt"], w=["lam4"])
            A("act", lambda e: e.activation(lam4[:, 2:4], lam4[:, 0:2], AF.Exp), r=["lam4"], w=["lam4"])
            A("dve", lambda e: e.tensor_tensor(lam4[:, 4:5], lam4[:, 3:4], lam4[:, 2:3], ALU.subtract), r=["lam4"], w=["lam4"])
            A("dve", lambda e, li_=lam_init: e.tensor_scalar(lam4[:, 5:6], lam4[:, 4:5], -li_, None, ALU.add), r=["lam4"], w=["lam4"])
            A("dve", lambda e, li_=lam_init: e.tensor_scalar(subg[:], subg[:], 1.0 - li_, None, ALU.mult), r=["subg"], w=["subg"])
            fence(["lamt"], ["TF3"])

            fence(["TF2"], ["zz", "carry"])
            for t in range(NT):
                for k in range(8):
                    A("pe", lambda e, t=t, k=k: e.matmul(pb[2][:, t * 8:(t + 1) * 8], hT[:, k, tsl(t)], wfz[:, k, :], start=(k == 0), stop=(k == 7)),
                      r=[("hT", t), "wfz"], w=[("pb", 2)])
            A("dve", lambda e: e.tensor_tensor(zz, pb[2][:, 0:128].rearrange("p (t h) -> p t h", h=8),
                                               ffb_bc[:].unsqueeze(1).to_broadcast([128, NT, 8]), ALU.add), r=[("pb", 2), "ffb"], w=["zz"])
            A("act", lambda e: e.activation(zz, zz, AF.Exp, scale=-1.0), r=["zz"], w=["zz"])
            A("act", lambda e: e.activation(zz, zz, AF.Ln, bias=one_t[:]), r=["zz", "one_t"], w=["zz"])
            zz2 = TFa[:, 2, 0:128]
            A("pe", lambda e: e.matmul(pb[3][:, 0:128], tri_f[:], zz2, start=True, stop=True), r=["zz", "tri_f"], w=[("pb", 3)])
            A("pe", lambda e: e.matmul(pb[3][:, 128:256], ones_f[:], zz2, start=True, stop=True), r=["zz", "ones_f"], w=[("pb", 3)])
            A("dve", lambda e: e.memset(carry[:, 0, :], 0.0), w=["carry"])
            A("dve", lambda e: e.tensor_copy(Gend[:], pb[3][:, 128:256].rearrange("p (t h) -> p t h", h=8)), r=[("pb", 3)], w=["Gend"])
            for t in range(1, NT):
                A("dve", lambda e, t=t: e.tensor_tensor(carry[:, t, :], carry[:, t - 1, :], Gend[:, t - 1, :], ALU.add), r=["carry", "Gend"], w=["carry"])
            A("dve", lambda e: e.tensor_tensor(Gc[:], pb[3][:, 0:128].rearrange("p (t h) -> p t h", h=8), carry, ALU.add),
              r=[("pb", 3), "carry"], w=["Gc"])
            A("pe", lambda e: e.matmul(pb[3][:, 256:384], sel_last[:], Gc[:].rearrange("p t h -> p (t h)"), start=True, stop=True),
              r=["Gc", "sel_last"], w=[("pb", 3)])
            A("dve", lambda e: e.tensor_copy(Gend[:], pb[3][:, 256:384].rearrange("p (t h) -> p t h", h=8)), r=[("pb", 3)], w=["Gend"])

            fence(["zz", "carry"], ["TF2"])
            if li > 0:
                fence(K_PLE16, K_POOLST)
                fence(K_MOEY, K_YT)
                fence(["WP"], K_PR)
            si = next_slot()
            wk = ("wsl", si)
            G.dma("pool", wsl[si][:].rearrange("p (k c) -> p k c", k=8), w_in_v[:, :, O_POOL:O_POOL + 512], w=[wk], slot=wk)
            wv = wsl[si][:].rearrange("p (k c) -> p k c", k=8)
            US = [[R16f[:, (st * 3 + i) * 528:(st * 3 + i + 1) * 528] for i in range(3)] for st in range(2)]
            DBS = [R16[:, 6400 + st * 512:6400 + (st + 1) * 512] for st in range(2)]
            for st in range(2):
                A("dve", lambda e, st=st: e.memset(US[st][1], 0.0), w=["U1_%d" % st])
                A("dve", lambda e, st=st: e.memset(US[st][2], 0.0), w=["U2_%d" % st])
            items = [(g, q) for g in range(4) for q in range(NQ)]

            def pool_M(i):
                g, q = items[i]
                bank = 4 + i % 2
                for k in range(8):
                    A("pe", lambda e, g=g, q=q, k=k, bank=bank, wv=wv: e.matmul(pb[bank], wv[:, k, g * 128:(g + 1) * 128], hT[:, k, qsl(q)], start=(k == 0), stop=(k == 7)),
                      r=[wk] + hT_all[4 * q:4 * q + 4], w=[("pb", bank)])

            def pool_CH(i):
                g, q = items[i]
                st = i % 2
                U = US[st]
                DB = DBS[st]
                ku = ["U0_%d" % st, "U1_%d" % st, "U2_%d" % st]
                kd = "DB_%d" % st
                wwin = 2 ** (g + 1)
                bank = 4 + i % 2
                if q == 0:
                    A("pool", lambda e, U=U: e.memset(U[0][:, 0:16], 0.0), w=[ku[0]])
                else:
                    A("pool", lambda e, U=U: e.tensor_copy(U[0][:, 0:16], halo[:]), r=["halo"], w=[ku[0]])
                A("act", lambda e, bank=bank, U=U: e.copy(U[0][:, 16:528], pb[bank]), r=[("pb", bank)], w=[ku[0]])
                A("pool", lambda e, U=U: e.tensor_copy(halo[:], U[0][:, 512:528]), r=[ku[0]], w=["halo"])
                src, srck = U[0], ku[0]
                sh = 1
                for step in range(g + 1):
                    dst, dstk = (U[1], ku[1]) if step % 2 == 0 else (U[2], ku[2])
                    A("dve", lambda e, src=src, dst=dst, sh=sh: e.tensor_tensor(dst[:, sh:528], src[:, sh:528], src[:, 0:528 - sh], ALU.add),
                      r=[srck], w=[dstk])
                    src, srck = dst, dstk
                    sh *= 2
                if q == 0:
                    A("dve", lambda e, src=src, g=g: e.tensor_tensor(src[:, 16:32], src[:, 16:32], ic16[:, g, :], ALU.mult), r=[srck, "ic16"], w=[srck])
                    A("dve", lambda e, src=src, U=U, DB=DB: e.tensor_tensor(DB[:, 0:16], src[:, 16:32], U[0][:, 16:32], ALU.subtract), r=[srck, ku[0]], w=[kd])
                    A("dve", lambda e, src=src, wwin=wwin, U=U, DB=DB: e.scalar_tensor_tensor(DB[:, 16:512], src[:, 32:528], 1.0 / wwin, U[0][:, 32:528], ALU.mult, ALU.subtract),
                      r=[srck, ku[0]], w=[kd])
                else:
                    A("dve", lambda e, src=src, wwin=wwin, U=U, DB=DB: e.scalar_tensor_tensor(DB[:, 0:512], src[:, 16:528], 1.0 / wwin, U[0][:, 16:528], ALU.mult, ALU.subtract),
                      r=[srck, ku[0]], w=[kd])

            def pool_Y(i):
                g, q = items[i]
                st = i % 2
                DB = DBS[st]
                yb = 6 + i % 2
                A("pe", lambda e, g=g, yb=yb, DB=DB: e.matmul(pb[yb], poolw[:, g, :], DB[:, 0:512], start=True, stop=True), r=["DB_%d" % st, "poolw"], w=[("pb", yb)])
                A("act", lambda e, g=g, q=q, yb=yb: e.activation(yT[:, g, qsl(q)], pb[yb], AF.Copy, scale=pools[:, g:g + 1]),
                  r=[("pb", yb), "pools"], w=[("yT", g, q)])

            pool_M(0)
            pool_CH(0)
            for i in range(len(items)):
                if i + 1 < len(items):
                    pool_M(i + 1)
                    pool_CH(i + 1)
                pool_Y(i)

            fence(K_POOLST, K_UNIT)
            QK = R16[:, 0:4096].rearrange("p (a s) -> p a s", a=2)
            Vu = R16[:, 4096:8192].rearrange("p (t c) -> p t c", t=NT)
            prc = [0]

            def next_pr():
                i = prc[0] % 8
                prc[0] += 1
                return i

            for u in range(8):
                is_fox = u >= 4
                j = u % 4
                oq, ok_, ov = (O_FQ, O_FK, O_FV) if is_fox else (O_DQ, O_DK, O_DV)
                si = next_slot()
                wk = ("wsl", si)
                wv = wsl[si][:, 0:3072].rearrange("p (k c) -> p k c", k=8)
                G.dma("pool", wv[:, :, 0:128], w_in_v[:, :, oq + j * 128:oq + (j + 1) * 128], w=[wk], slot=wk, nfill=3)
                G.dma("pool", wv[:, :, 128:256], w_in_v[:, :, ok_ + j * 128:ok_ + (j + 1) * 128], w=[wk], slot=wk, cont=True)
                G.dma("pool", wv[:, :, 256:384], w_in_v[:, :, ov + j * 128:ov + (j + 1) * 128], w=[wk], slot=wk, cont=True)
                gk = "qkg1" if is_fox else "qkg0"
                gt = qkg[1] if is_fox else qkg[0]
                if u == 4:
                    A("dve", lambda e: e.memset(Vu[:, :, 64:192], 1.0), w=[("Vu", t) for t in range(NT)])
                if is_fox:
                    for hh in range(2):
                        h = 2 * j + hh
                        A("dve", lambda e, hh=hh, h=h: e.tensor_tensor(biasu[:, hh, :, 0:8], Gc[:, :, h].unsqueeze(2).to_broadcast([128, NT, 8]),
                                                                      Gend[:].rearrange("p (a b) h -> p a b h", b=2)[:, :, 1, h].unsqueeze(1).to_broadcast([128, NT, 8]), ALU.subtract),
                          r=["Gc", "Gend"], w=["biasu"])
                SQ = TFa[:, 0:2, :].rearrange("p a c -> p (a c)")
                QN = TFa[:, 2:4, :].rearrange("p a c -> p (a c)")
                def b2_geom(tg):
                    s4 = 4 * (tg % 2)
                    return s4, [s4, s4 + 1], s4 + 2, s4 + 3, list(range(4 * tg, 4 * tg + 4)), TB[tg % 2], ("TB", tg % 2)

                def emit_Mqk(tg):
                    s4, bQ, bV, bT, tiles, tb, kt = b2_geom(tg)
                    for tl, t in enumerate(tiles):
                        for k in range(8):
                            A("pe", lambda e, t=t, k=k, tl=tl, bQ=bQ, wv=wv: e.matmul(pb[bQ[tl // 2]][:, (tl % 2) * 256:(tl % 2) * 256 + 256], hT[:, k, tsl(t)], wv[:, k, 0:256], start=(k == 0), stop=(k == 7)),
                              r=[("hT", t), wk], w=[("pb", bQ[tl // 2])])

                def emit_Mv(tg):
                    s4, bQ, bV, bT, tiles, tb, kt = b2_geom(tg)
                    for tl, t in enumerate(tiles):
                        for k in range(8):
                            A("pe", lambda e, t=t, k=k, tl=tl, bV=bV, wv=wv: e.matmul(pb[bV][:, tl * 128:(tl + 1) * 128], hT[:, k, tsl(t)], wv[:, k, 256:384], start=(k == 0), stop=(k == 7)),
                              r=[("hT", t), wk], w=[("pb", bV)])

                PRf = PR[:].rearrange("p a c -> p (a c)").bitcast(F32)

                def emit_CH(tg):
                    s4, bQ, bV, bT, tiles, tb, kt = b2_geom(tg)
                    if tg % 2 == 0:
                        SQb = TFa[:, 0, :].bitcast(BF16)
                        kSQ = ["TF0"]
                        QNx = TFa[:, 2:4, :].rearrange("p a c -> p (a c)")
                        kQN = ["TF2", "TF3"]
                        R4 = TFa[:, 1, :]
                        kR4 = ["TF1"]
                        so = 0
                    else:
                        SQb = PR[:, 0:2, :].rearrange("p a c -> p (a c)")
                        kSQ = [("PR", 0), ("PR", 1)]
                        QNx = PRf[:, 1024:2048]
                        kQN = [("PR", 4), ("PR", 5), ("PR", 6), ("PR", 7)]
                        R4 = PRf[:, 512:1024]
                        kR4 = [("PR", 2), ("PR", 3)]
                        so = 48
                    ka, kb_, kc = "sm_a%d" % so, "sm_b%d" % so, "sm_c%d" % so
                    PQ = PSa[:, s4:s4 + 2, :].rearrange("p b c -> p (b c)")
                    kq = [("pb", bQ[0]), ("pb", bQ[1])]
                    A("act", lambda e, PQ=PQ, SQb=SQb: e.activation(SQb, PQ, AF.Square), r=kq, w=kSQ)
                    A("dve", lambda e, SQb=SQb, so=so: e.tensor_reduce(sm[:, so:so + 16], SQb.rearrange("p (h d) -> p h d", d=64), AX.X, ALU.add), r=kSQ, w=[ka])
                    A("act", lambda e, so=so: e.activation(sm[:, so + 16:so + 32], sm[:, so:so + 16], AF.Ln, bias=eps_t[:], scale=1.0 / 64), r=[ka, "eps_t"], w=[kb_])
                    A("act", lambda e, so=so: e.activation(sm[:, so + 32:so + 48], sm[:, so + 16:so + 32], AF.Exp, scale=-0.5), r=[kb_], w=[kc])
                    A("dve", lambda e, PQ=PQ, QNx=QNx, so=so: e.tensor_tensor(QNx.rearrange("p (h d) -> p h d", d=64), PQ.rearrange("p (h d) -> p h d", d=64),
                                                                               sm[:, so + 32:so + 48].unsqueeze(2).to_broadcast([128, 16, 64]), ALU.mult), r=kq + [kc], w=kQN)
                    gb = gt[:].unsqueeze(1).to_broadcast([128, 4, 256])
                    if is_fox:
                        A("dve", lambda e, tb=tb, gb=gb, QNx=QNx: e.tensor_tensor(tb[:].rearrange("p (t c) -> p t c", t=4), QNx.rearrange("p (t c) -> p t c", t=4), gb, ALU.mult),
                          r=kQN + [gk], w=[kt])
                    else:
                        A("dve", lambda e, gb=gb, QNx=QNx: e.tensor_tensor(QNx.rearrange("p (t c) -> p t c", t=4), QNx.rearrange("p (t c) -> p t c", t=4), gb, ALU.mult),
                          r=kQN + [gk], w=kQN)
                        A("act", lambda e, tb=tb, QNx=QNx: e.copy(tb[:], QNx), r=kQN, w=[kt])
                        q4 = QNx.rearrange("p (t h d) -> p t h d", t=4, h=4)
                        o4 = tb[:].rearrange("p (t h d) -> p t h d", t=4, h=4)
                        r4 = R4.rearrange("p (a t h d) -> p a t h d", a=4, t=4, h=4)
                        cb = cos_t[:, 4 * tg:4 * tg + 4, :].unsqueeze(2).to_broadcast([128, 4, 4, 8])
                        sbb = sin_t[:, 4 * tg:4 * tg + 4, :].unsqueeze(2).to_broadcast([128, 4, 4, 8])
                        A("dve", lambda e, q4=q4, r4=r4, cb=cb: e.tensor_tensor(r4[:, 0], q4[:, :, :, 0:8], cb, ALU.mult), r=kQN + ["cos_t"], w=kR4)
                        A("dve", lambda e, q4=q4, r4=r4, sbb=sbb: e.tensor_tensor(r4[:, 1], q4[:, :, :, 8:16], sbb, ALU.mult), r=kQN + ["sin_t"], w=kR4)
                        A("dve", lambda e, q4=q4, r4=r4, cb=cb: e.tensor_tensor(r4[:, 2], q4[:, :, :, 8:16], cb, ALU.mult), r=kQN + ["cos_t"], w=kR4)
                        A("dve", lambda e, q4=q4, r4=r4, sbb=sbb: e.tensor_tensor(r4[:, 3], q4[:, :, :, 0:8], sbb, ALU.mult), r=kQN + ["sin_t"], w=kR4)
                        A("dve", lambda e, o4=o4, r4=r4: e.tensor_tensor(o4[:, :, :, 0:8], r4[:, 0], r4[:, 1], ALU.subtract), r=kR4 + [kt], w=[kt])
                        A("dve", lambda e, o4=o4, r4=r4: e.tensor_tensor(o4[:, :, :, 8:16], r4[:, 2], r4[:, 3], ALU.add), r=kR4 + [kt], w=[kt])

                def emit_T(tg):
                    s4, bQ, bV, bT, tiles, tb, kt = b2_geom(tg)
                    for tl in range(4):
                        for a in range(2):
                            A("pe", lambda e, a=a, tl=tl, tb=tb, bT=bT: e.transpose(pbb(bT)[:, a * 512 + tl * 128:a * 512 + (tl + 1) * 128], tb[:, tl * 256 + a * 128:tl * 256 + (a + 1) * 128], ident_b[:]),
                              r=[kt, "ident_b"], w=[("pb", bT)])
                    A("act", lambda e, tg=tg, bT=bT: e.copy(QK[:, :, tg * 512:(tg + 1) * 512], pbb(bT).rearrange("p (a c) -> p a c", a=2)),
                      r=[("pb", bT)], w=[("QK", t) for t in tiles])
                    pv = pb[bV].rearrange("p (t c) -> p t c", t=4)
                    if is_fox:
                        A("act", lambda e, tg=tg, pv=pv: e.copy(Vu[:, 4 * tg:4 * tg + 4, 0:64], pv[:, :, 0:64]), r=[("pb", bV)], w=[("Vu", t) for t in tiles])
                        A("act", lambda e, tg=tg, pv=pv: e.copy(Vu[:, 4 * tg:4 * tg + 4, 192:256], pv[:, :, 64:128]), r=[("pb", bV)], w=[("Vu", t) for t in tiles])
                    else:
                        A("act", lambda e, tg=tg, pv=pv: e.copy(Vu[:, 4 * tg:4 * tg + 4, 0:128], pv), r=[("pb", bV)], w=[("Vu", t) for t in tiles])


                emit_Mqk(0)
                emit_Mv(0)
                flush()
                emit_CH(0)
                emit_Mqk(1)
                emit_Mv(1)
                emit_CH(1)
                emit_Mqk(2)
                emit_T(0)
                emit_Mv(2)
                emit_CH(2)
                emit_Mqk(3)
                emit_T(1)
                emit_Mv(3)
                emit_CH(3)
                emit_T(2)
                emit_T(3)

                steps = [(q, kb) for q in range(NQ) for kb in range(4 * (q + 1))]

                def geom(q, kb):
                    jd = kb - 4 * q
                    c0 = max(0, jd) * 128
                    return jd, c0, 512 - c0

                def emit_S(q, kb):
                    jd, c0, N = geom(q, kb)
                    sbk = [(kb % 2) * 2, (kb % 2) * 2 + 1]
                    qk_keys = [("QK", t) for t in range(4 * q, 4 * q + 4)]
                    for a in range(2):
                        pa = slice(a * 64, (a + 1) * 64)
                        A("pe", lambda e, a=a, pa=pa, kb=kb, c0=c0, N=N, q=q, sbk=sbk: e.matmul(pb[sbk[a]][:, 0:N], QK[pa, 1, tsl(kb)], QK[pa, 0, q * 512 + c0:(q + 1) * 512], start=True, stop=True),
                          r=[("QK", kb)] + qk_keys, w=[("pb", sbk[a])])

                def emit_E(q, kb):
                    jd, c0, N = geom(q, kb)
                    sbk = [(kb % 2) * 2, (kb % 2) * 2 + 1]
                    prs = []
                    for a in range(2):
                        pi = next_pr()
                        prs.append(pi)
                        if not is_fox:
                            A("act", lambda e, a=a, pi=pi, N=N, sbk=sbk: e.activation(PR[:, pi, 0:N], pb[sbk[a]][:, 0:N], AF.Exp, scale=0.125),
                              r=[("pb", sbk[a])], w=[("PR", pi)])
                        else:
                            for s2 in range(2):
                                lo = max(c0, s2 * 256)
                                hi = (s2 + 1) * 256
                                if lo >= hi:
                                    continue
                                qb2 = 2 * q + s2
                                A("act", lambda e, a=a, pi=pi, lo=lo, hi=hi, c0=c0, kb=kb, qb2=qb2, sbk=sbk: e.activation(PR[:, pi, lo - c0:hi - c0], pb[sbk[a]][:, lo - c0:hi - c0], AF.Exp,
                                                                                                           bias=biasu[:, a, kb, qb2:qb2 + 1], scale=0.125),
                                  r=[("pb", sbk[a]), "biasu"], w=[("PR", pi)])
                        if jd >= 0:
                            A("dve", lambda e, pi=pi: e.tensor_tensor(PR[:, pi, 0:128], PR[:, pi, 0:128], tri_b[:], ALU.mult), r=[("PR", pi), "tri_b"], w=[("PR", pi)])
                    return prs

                def emit_AV(q, kb, prs):
                    jd, c0, N = geom(q, kb)
                    nkb = 4 * (q + 1)
                    if not is_fox:
                        for a in range(2):
                            ob = 4 + a
                            lb = 6 + a
                            pi = prs[a]
                            A("pe", lambda e, ob=ob, pi=pi, kb=kb, c0=c0, N=N, nkb=nkb: e.matmul(pb[ob][:, c0:512], Vu[:, kb, 0:128], PR[:, pi, 0:N], start=(kb == 0), stop=(kb == nkb - 1)),
                              r=[("PR", pi), ("Vu", kb)], w=[("pb", ob)])
                            A("pe", lambda e, lb=lb, pi=pi, kb=kb, c0=c0, N=N, nkb=nkb: e.matmul(pb[lb][:, c0:512], ones_b[:], PR[:, pi, 0:N], start=(kb == 0), stop=(kb == nkb - 1)),
                              r=[("PR", pi), "ones_b"], w=[("pb", lb)])
                    else:
                        for a in range(2):
                            ob = 4 + 2 * (q % 2) + a
                            pi = prs[a]
                            A("pe", lambda e, a=a, ob=ob, pi=pi, kb=kb, c0=c0, N=N, nkb=nkb: e.matmul(pb[ob][:, c0:512], Vu[:, kb, a * 128:(a + 1) * 128], PR[:, pi, 0:N], start=(kb == 0), stop=(kb == nkb - 1)),
                              r=[("PR", pi), ("Vu", kb)], w=[("pb", ob)])

                def emit_post_a(q):
                    if not is_fox:
                        O1, O2, L1, L2 = 4, 5, 6, 7
                        A("act", lambda e: e.activation(TF[2], pb[L1], AF.Ln), r=[("pb", L1)], w=["TF2"])
                        A("act", lambda e: e.activation(TF[3], pb[L2], AF.Ln), r=[("pb", L2)], w=["TF3"])
                        A("dve", lambda e: e.tensor_copy(TF[0], pb[O1]), r=[("pb", O1)], w=["TF0"])
                        A("dve", lambda e: e.tensor_copy(TF[1], pb[O2]), r=[("pb", O2)], w=["TF1"])
                    else:
                        OA = 4 + 2 * (q % 2)
                        OB = 5 + 2 * (q % 2)
                        tf = TF[q % 2]
                        tk = "TF%d" % (q % 2)
                        A("dve", lambda e, OA=OA, tf=tf: e.reciprocal(tf[0:64, :], pb[OA][64:128, :]), r=[("pb", OA)], w=[tk])
                        A("dve", lambda e, OB=OB, tf=tf: e.reciprocal(tf[64:128, :], pb[OB][0:64, :]), r=[("pb", OB)], w=[tk])
                        A("dve", lambda e, OA=OA, q=q, tf=tf, j=j: e.tensor_tensor(yT[0:64, 8 + j, qsl(q)], pb[OA][0:64, :], tf[0:64, :], ALU.mult),
                          r=[("pb", OA), tk], w=[("yT", 8 + j, q)])
                        A("dve", lambda e, OB=OB, q=q, tf=tf, j=j: e.tensor_tensor(yT[64:128, 8 + j, qsl(q)], pb[OB][64:128, :], tf[64:128, :], ALU.mult),
                          r=[("pb", OB), tk], w=[("yTb", 8 + j, q)])

                def emit_post_b(q, bk):
                    if is_fox:
                        return
                    A("act", lambda e: e.activation(TF[2], TF[2], AF.Exp, scale=-1.0), r=["TF2"], w=["TF2"])
                    A("act", lambda e: e.activation(TF[3], TF[3], AF.Exp, scale=-1.0), r=["TF3"], w=["TF3"])
                    A("dve", lambda e: e.tensor_tensor(TF[0], TF[0], TF[2], ALU.mult), r=["TF0", "TF2"], w=["TF0"])
                    A("dve", lambda e: e.tensor_tensor(TF[1], TF[1], TF[3], ALU.mult), r=["TF1", "TF3"], w=["TF1"])
                    A("dve", lambda e: e.scalar_tensor_tensor(TF[0], TF[1], lam4[:, 5:6], TF[0], ALU.mult, ALU.add), r=["TF0", "TF1", "lam4"], w=["TF0"])
                    A("act", lambda e: e.activation(TB[0][:, 0:512], TF[0], AF.Square), r=["TF0"], w=[("TB", 0)])
                    A("pe", lambda e, bk=bk: e.matmul(pb[bk], ones_b[:], TB[0][:, 0:512], start=True, stop=True), r=[("TB", 0), "ones_b"], w=[("pb", bk)])
                    A("act", lambda e, bk=bk: e.activation(TF[1], pb[bk], AF.Ln, bias=eps_t[:], scale=1.0 / 128), r=[("pb", bk), "eps_t"], w=["TF1"])
                    A("act", lambda e: e.activation(TF[1], TF[1], AF.Exp, scale=-0.5), r=["TF1"], w=["TF1"])
                    A("dve", lambda e, q=q, j=j: e.scalar_tensor_tensor(yT[:, 4 + j, qsl(q)], TF[0], subg[:, 0:1], TF[1], ALU.mult, ALU.mult),
                      r=["TF0", "TF1", "subg"], w=[("yT", 4 + j, q)])

                emit_S(*steps[0])
                pend_a = None
                pend_b = None
                for i, (q, kb) in enumerate(steps):
                    if i + 1 < len(steps):
                        emit_S(*steps[i + 1])
                    prs = emit_E(q, kb)
                    if pend_a is not None:
                        emit_post_a(pend_a)
                        pend_b = (pend_a, i + 2)
                        pend_a = None
                    emit_AV(q, kb, prs)
                    if pend_b is not None and i >= pend_b[1]:
                        emit_post_b(pend_b[0], (kb % 2) * 2 + 1)
                        pend_b = None
                    if kb == 4 * (q + 1) - 1:
                        pend_a = q
                defer[0] = True
                if pend_b is not None:
                    emit_post_b(pend_b[0], 3)
                emit_post_a(pend_a)
                emit_post_b(pend_a, 7)
                defer[0] = False

            flush()
            fence(K_UNIT, K_M4)
            fence(K_PR, ["WO"])
            M4 = R16[:].rearrange("p (c s) -> p c s", c=4)
            WO = PR[:].rearrange("p a c -> p (a c)").rearrange("p (c d) -> p c d", c=4)
            yT_keys = lambda n, k, q: [("yT", n * 4 + k, q)] + ([("yTb", n * 4 + k, q)] if n == 2 else [])
            for half in range(2):
                G.dma("pool", WO, w_out_d[li].rearrange("(k p) d -> p k d", p=128)[:, 4 * half:4 * half + 4, :], w=["WO"], slot="WO")
                for dc in range(4):
                    dch = 4 * half + dc
                    for n in range(3):
                        si = next_slot()
                        wk = ("wsl", si)
                        wv = wsl[si][:, 0:1536].rearrange("p (k c) -> p k c", k=12)
                        G.dma("pool", wv[:, 0:8, :], w_in_v[:, :, O_GZ + n * D + dch * 128:O_GZ + n * D + (dch + 1) * 128], w=[wk], slot=wk, nfill=2)
                        G.dma("pool", wv[:, 8:12, :], w_br_d[li, n].rearrange("(k p) d -> p k d", p=128)[:, :, dch * 128:(dch + 1) * 128], w=[wk], slot=wk, cont=True)
                        for q in range(NQ):
                            bg = 2 * (q % 2)
                            br_ = 2 * (q % 2) + 1
                            for k in range(8):
                                A("pe", lambda e, k=k, q=q, bg=bg, wv=wv: e.matmul(pb[bg], wv[:, k, :], hT[:, k, qsl(q)], start=(k == 0), stop=(k == 7)),
                                  r=[wk] + hT_all[4 * q:4 * q + 4], w=[("pb", bg)])
                            for k in range(4):
                                A("pe", lambda e, k=k, q=q, br_=br_, n=n, wv=wv: e.matmul(pb[br_], wv[:, 8 + k, :], yT[:, n * 4 + k, qsl(q)], start=(k == 0), stop=(k == 3)),
                                  r=[wk] + yT_keys(n, k, q), w=[("pb", br_)])
                            sg = TB[q % 2]
                            sgk = ("TB", q % 2)
                            A("act", lambda e, sg=sg, bg=bg: e.activation(sg[:, 0:512], pb[bg], AF.Sigmoid), r=[("pb", bg)], w=[sgk])
                            mk = "TF%d" % q
                            if n == 0:
                                A("dve", lambda e, q=q, sg=sg, br_=br_: e.tensor_tensor(TF[q], sg[:, 0:512], pb[br_], ALU.mult), r=[sgk, ("pb", br_)], w=[mk])
                            else:
                                A("dve", lambda e, sg=sg, br_=br_: e.tensor_tensor(sg[:, 512:1024], sg[:, 0:512], pb[br_], ALU.mult), r=[sgk, ("pb", br_)], w=[sgk])
                                if n == 1:
                                    A("dve", lambda e, q=q, sg=sg: e.tensor_tensor(TF[q], TF[q], sg[:, 512:1024], ALU.add), r=[sgk, mk], w=[mk])
                                else:
                                    A("dve", lambda e, q=q, sg=sg, dc=dc: e.tensor_tensor(M4[:, dc, qsl(q)], TF[q], sg[:, 512:1024], ALU.add), r=[sgk, mk], w=[("M4", dc, q)])
                for t in range(NT):
                    for cg in range(2):
                        bank = 4 + (t * 2 + cg) % 4
                        for dc in range(4):
                            A("pe", lambda e, t=t, cg=cg, dc=dc, bank=bank: e.matmul(pb[bank], M4[:, dc, tsl(t)], WO[:, dc, cg * 512:(cg + 1) * 512], start=(dc == 0), stop=(dc == 3)),
                              r=[("M4", dc, t // 4), "WO"], w=[("pb", bank)])
                        A("dve", lambda e, t=t, cg=cg, bank=bank: e.tensor_tensor(x_sb[:, t, cg * 512:(cg + 1) * 512], x_sb[:, t, cg * 512:(cg + 1) * 512], pb[bank], ALU.add),
                          r=[("pb", bank), ("x", t)], w=[("x", t)])

            rmsnorm_to_hT(ffn_g_d[li])
            G.dma("pool", w_r[:, :, 0:4], wrg_d[li].rearrange("(k p) c -> p k c", p=128), w=["w_r"], slot="w_r", nfill=2)
            G.dma("pool", w_r[:, :, 4:20], wre_d[li].rearrange("(k p) c -> p k c", p=128), w=["w_r"], slot="w_r", cont=True)
            G.dma("sp", b_r[:, 0:4], brg_d[li].partition_broadcast(128), w=["b_r"], slot="b_r", nfill=2)
            G.dma("sp", b_r[:, 4:20], bre_d[li].partition_broadcast(128), w=["b_r"], slot="b_r", cont=True)
            for t in range(NT):
                for k in range(8):
                    A("pe", lambda e, t=t, k=k: e.matmul(pb[0][:, t * 20:(t + 1) * 20], hT[:, k, tsl(t)], w_r[:, k, :], start=(k == 0), stop=(k == 7)),
                      r=[("hT", t), "w_r"], w=[("pb", 0)])
            fence(["TF2", "TF3"], ["LE", "RA", "RB", "comb"])
            K_RS = ["LG", "RP", "gmax", "gsum", "m1", "m2", "wsum", "coef"]
            fence([("TB", 1)], K_RS)
            pl = pb[0][:, 0:320].rearrange("p (t c) -> p t c", c=20)
            A("dve", lambda e: e.tensor_tensor(LG, pl[:, :, 0:4], b_r[:, 0:4].unsqueeze(1).to_broadcast([128, NT, 4]), ALU.add), r=[("pb", 0), "b_r"], w=["LG"])
            A("dve", lambda e: e.tensor_tensor(LE, pl[:, :, 4:20], b_r[:, 4:20].unsqueeze(1).to_broadcast([128, NT, 16]), ALU.add), r=[("pb", 0), "b_r"], w=["LE"])
            bc4 = lambda ap: ap.unsqueeze(2).to_broadcast([128, NT, 4])
            bc16 = lambda ap: ap.unsqueeze(2).to_broadcast([128, NT, 16])
            gmax, gsum, m1, m2, wsum, coef = (RS[:, i, :] for i in range(6))
            A("dve", lambda e: e.tensor_reduce(gmax, LG, AX.X, ALU.max), r=["LG"], w=["gmax"])
            A("dve", lambda e: e.tensor_tensor(RA[:, :, 0:4], LG, bc4(gmax), ALU.subtract), r=["LG", "gmax"], w=["RA"])
            A("act", lambda e: e.activation(RA[:, :, 0:4], RA[:, :, 0:4], AF.Exp), r=["RA"], w=["RA"])
            A("dve", lambda e: e.tensor_reduce(gsum, RA[:, :, 0:4], AX.X, ALU.add), r=["RA"], w=["gsum"])
            A("dve", lambda e: e.tensor_tensor(RP, LG, bc4(gmax), ALU.is_ge), r=["LG", "gmax"], w=["RP"])
            A("dve", lambda e: e.tensor_scalar(RP, RP, 1.0, BIG, ALU.subtract, ALU.mult), r=["RP"], w=["RP"])
            A("dve", lambda e: e.tensor_tensor(LE.rearrange("p t (g x) -> p (t g) x", g=4), LE.rearrange("p t (g x) -> p (t g) x", g=4),
                                               RP.rearrange("p t g -> p (t g)").unsqueeze(2).to_broadcast([128, NT * 4, 4]), ALU.add),
              r=["LE", "RP"], w=["LE"])
            A("dve", lambda e: e.tensor_reduce(m1, LE, AX.X, ALU.max), r=["LE"], w=["m1"])
            A("dve", lambda e: e.tensor_tensor(RB, LE, bc16(m1), ALU.is_ge), r=["LE", "m1"], w=["RB"])
            A("dve", lambda e: e.scalar_tensor_tensor(RB, RB, -BIG, LE, ALU.mult, ALU.add), r=["RB", "LE"], w=["RB"])
            A("dve", lambda e: e.tensor_reduce(m2, RB, AX.X, ALU.max), r=["RB"], w=["m2"])
            A("dve", lambda e: e.tensor_tensor(RB, LE, bc16(m2), ALU.is_ge), r=["LE", "m2"], w=["RB"])
            A("dve", lambda e: e.tensor_tensor(RA, LE, bc16(m1), ALU.subtract), r=["LE", "m1"], w=["RA"])
            A("act", lambda e: e.activation(RA, RA, AF.Exp), r=["RA"], w=["RA"])
            A("dve", lambda e: e.tensor_tensor(RA, RA, RB, ALU.mult), r=["RA", "RB"], w=["RA"])
            A("dve", lambda e: e.tensor_reduce(wsum, RA, AX.X, ALU.add), r=["RA"], w=["wsum"])
            A("dve", lambda e: e.tensor_tensor(coef, wsum, gsum, ALU.mult), r=["wsum", "gsum"], w=["coef"])
            A("dve", lambda e: e.reciprocal(coef, coef), r=["coef"], w=["coef"])
            A("dve", lambda e: e.tensor_tensor(comb, RA, bc16(coef), ALU.mult), r=["RA", "coef"], w=["comb"])
            fence(K_M4, K_MOE16)
            fence(K_YT, K_MOEY)
            combT = R16f[0:16, 0:2048]
            selE = R16f[0:16, 2048:4096].rearrange("p (e m) -> p e m", e=16)
            A("pool", lambda e: e.memset(R16f[0:16, 2048:4096], 1.0), w=["selE"])
            A("pool", lambda e: e.affine_select(R16f[0:16, 2048:4096], R16f[0:16, 2048:4096], [[-1, 16], [0, 128]], ALU.is_equal, 0.0, base=0, channel_multiplier=1),
              r=["selE"], w=["selE"])
            for t in range(NT):
                bank = 1 + t // 4
                A("pe", lambda e, t=t, bank=bank: e.transpose(pb[bank][0:16, (t % 4) * 128:(t % 4 + 1) * 128], comb[:, t, :], ident_f[:]),
                  r=["comb", "ident_f"], w=[("pb", bank)])
            for i in range(4):
                A("dve", lambda e, i=i: e.tensor_copy(combT[:, qsl(i)], pb[1 + i][0:16, :]), r=[("pb", 1 + i)], w=["combT"])
            fence(["LE", "RA", "RB", "comb"], ["TF2", "TF3"])
            fence(K_RS, [("TB", 1)])
            G.dma("sp", comb_scr, combT, r=["combT"], w=["comb_scr"], slot="comb_scr")
            yTf = yT[:].rearrange("p a s -> p (a s)")
            ACT4 = yTf[:, 0:16384].rearrange("p (c s) -> p c s", c=8)
            WD = yTf[:, 16384:24576].rearrange("p (e f d) -> p e f d", e=4, f=2)
            for g4 in range(4):
                for e4 in range(4):
                    ex = 4 * g4 + e4
                    si = next_slot()
                    wk = ("wsl", si)
                    wv = wsl[si][:].rearrange("p (k c) -> p k c", k=8)
                    G.dma("pool", wv[:, :, 0:256], mg_d[li, ex].rearrange("(k p) f -> p k f", p=128), w=[wk], slot=wk, nfill=2)
                    G.dma("pool", wv[:, :, 256:512], mu_d[li, ex].rearrange("(k p) f -> p k f", p=128), w=[wk], slot=wk, cont=True)
                    G.dma("pool", WD[:, e4], md_d[li, ex].rearrange("(f p) d -> p f d", p=128), w=[("WD", e4)], slot=("WD", e4))
                    for q in range(NQ):
                        cs = 2 + (ex * NQ + q) % 2
                        ck = "TF%d" % cs
                        G.dma("sp", TF[cs], comb_scr[ex, q * 512:(q + 1) * 512].partition_broadcast(128), r=["comb_scr"], w=[ck], slot=("cb", cs))
                        for fc in range(2):
                            bg = 2 * fc
                            bu = 2 * fc + 1
                            for k in range(8):
                                A("pe", lambda e, k=k, q=q, fc=fc, bg=bg, wv=wv: e.matmul(pb[bg], wv[:, k, fc * 128:(fc + 1) * 128], hT[:, k, qsl(q)], start=(k == 0), stop=(k == 7)),
                                  r=[wk] + hT_all[4 * q:4 * q + 4], w=[("pb", bg)])
                            for k in range(8):
                                A("pe", lambda e, k=k, q=q, fc=fc, bu=bu, wv=wv: e.matmul(pb[bu], wv[:, k, 256 + fc * 128:256 + (fc + 1) * 128], hT[:, k, qsl(q)], start=(k == 0), stop=(k == 7)),
                                  r=[wk] + hT_all[4 * q:4 * q + 4], w=[("pb", bu)])
                            A("act", lambda e, fc=fc, bg=bg: e.activation(TF[fc], pb[bg], AF.Silu), r=[("pb", bg)], w=["TF%d" % fc])
                            A("dve", lambda e, fc=fc, bu=bu: e.tensor_tensor(TF[fc], TF[fc], pb[bu], ALU.mult), r=["TF%d" % fc, ("pb", bu)], w=["TF%d" % fc])
                            A("dve", lambda e, fc=fc, e4=e4, q=q, cs=cs: e.tensor_tensor(ACT4[:, e4 * 2 + fc, qsl(q)], TF[fc], TF[cs], ALU.mult),
                              r=["TF%d" % fc, ck], w=[("ACT4", e4 * 2 + fc, q)])
                for t in range(NT):
                    for cg in range(2):
                        bank = 4 + (t * 2 + cg) % 4
                        for c in range(8):
                            A("pe", lambda e, t=t, cg=cg, c=c, bank=bank: e.matmul(pb[bank], ACT4[:, c, tsl(t)], WD[:, c // 2, c % 2, cg * 512:(cg + 1) * 512], start=(c == 0), stop=(c == 7)),
                              r=[("ACT4", c, t // 4), ("WD", c // 2)], w=[("pb", bank)])
                        A("dve", lambda e, t=t, cg=cg, bank=bank: e.tensor_tensor(x_sb[:, t, cg * 512:(cg + 1) * 512], x_sb[:, t, cg * 512:(cg + 1) * 512], pb[bank], ALU.add),
                          r=[("pb", bank), ("x", t)], w=[("x", t)])

            rmsnorm_to_hT(ple_g_d[li])
            PB = R16[:, 0:4096].rearrange("p (t c) -> p t c", t=NT)
            PT = R16[:, 4096:8192].rearrange("p (k s) -> p k s", k=2)
            WP = PR[:].rearrange("p a c -> p (a c)")[:, 0:2048].rearrange("p (k d) -> p k d", k=2)
            fence(K_MOE16, K_PLE16)
            fence(["WO"], ["WP"])
            G.dma("pool", PB, p_d[li].rearrange("(t p) c -> p t c", p=128), w=["PB"], slot="PB")
            G.dma("pool", WP, wp_d[li].rearrange("(k p) d -> p k d", p=128), w=["WP"], slot="WP")
            for t in range(NT):
                bank = 2 + t % 2
                for k in range(2):
                    A("pe", lambda e, t=t, k=k, bank=bank: e.transpose(pbb(bank)[:, k * 128:(k + 1) * 128], PB[:, t, k * 128:(k + 1) * 128], ident_b[:]),
                      r=["PB", "ident_b"], w=[("pb", bank)])
                A("act", lambda e, t=t, bank=bank: e.copy(PT[:, :, tsl(t)], pbb(bank)[:, 0:256].rearrange("p (k c) -> p k c", k=2)),
                  r=[("pb", bank)], w=[("PT", t)])
            for cg in range(2):
                si = next_slot()
                wk = ("wsl", si)
                wv = wsl[si][:].rearrange("p (k c) -> p k c", k=8)
                G.dma("pool", wv, wpg_d[li].rearrange("(k p) c -> p k c", p=128)[:, :, cg * 512:(cg + 1) * 512], w=[wk], slot=wk)
                for t in range(NT):
                    ba = 4 + 2 * (t % 2)
                    bb_ = 5 + 2 * (t % 2)
                    for k in range(8):
                        A("pe", lambda e, t=t, k=k, ba=ba, wv=wv: e.matmul(pb[ba], hT[:, k, tsl(t)], wv[:, k, :], start=(k == 0), stop=(k == 7)),
                          r=[("hT", t), wk], w=[("pb", ba)])
                    for k in range(2):
                        A("pe", lambda e, t=t, k=k, bb_=bb_, cg=cg: e.matmul(pb[bb_], PT[:, k, tsl(t)], WP[:, k, cg * 512:(cg + 1) * 512], start=(k == 0), stop=(k == 1)),
                          r=[("PT", t), "WP"], w=[("pb", bb_)])
                    tf = TF[t % 2]
                    tk = "TF%d" % (t % 2)
                    A("act", lambda e, tf=tf, ba=ba: e.activation(tf[:], pb[ba], AF.Sigmoid), r=[("pb", ba)], w=[tk])
                    A("dve", lambda e, tf=tf, bb_=bb_: e.tensor_tensor(tf[:], tf[:], pb[bb_], ALU.mult), r=[tk, ("pb", bb_)], w=[tk])
                    A("dve", lambda e, t=t, cg=cg, tf=tf: e.tensor_tensor(x_sb[:, t, cg * 512:(cg + 1) * 512], x_sb[:, t, cg * 512:(cg + 1) * 512], tf[:], ALU.add),
                      r=[tk, ("x", t)], w=[("x", t)])

        ov = out_d.rearrange("(t p) d -> p t d", p=128)
        for i, (t0, t1) in enumerate([(0, 4), (4, 8), (8, 12), (12, 14), (14, 15), (15, 16)]):
            G.dma("sp", ov[:, t0:t1, :], x_sb[:, t0:t1, :], r=[("x", t) for t in range(t0, t1)], slot=("xs", i))
        with nc.allow_non_contiguous_dma(reason="tiny per-layer parameter vectors"):
            G.emit()
    return nc


W_NAMES = ["attn_norm_g", "w_in", "pool_w", "pool_scale", "diff_qn_g", "diff_kn_g", "diff_lambda", "diff_subln_g",
           "fox_qn_g", "fox_kn_g", "fox_forget_b", "w_branch", "w_out", "ffn_norm_g", "w_route_group", "b_route_group",
           "w_route_expert", "b_route_expert", "moe_w_gate", "moe_w_up", "moe_w_down", "ple_norm_g", "w_ple_gate", "w_ple"]

_CACHE = {}
FUSED = True


def _prog(NL, layer0):
    key = (NL, layer0)
    if key not in _CACHE:
        _CACHE[key] = build_program(NL, layer0)
    return _CACHE[key]


def _run(xin, inputs, layers):
    NL = len(layers)
    l0 = layers[0]
    nc = _prog(NL, l0)
    shared = {}
    for n in W_NAMES:
        a = np.asarray(inputs[n], dtype=np.float32)[l0:l0 + NL]
        if n == "diff_lambda":
            a = a.reshape(NL, 256)
        shared[n] = np.ascontiguousarray(a)
    p = np.asarray(inputs["p"], dtype=np.float32)
    pos = np.asarray(inputs["positions"], dtype=np.int32)
    in_maps = []
    for b in range(8):
        m = dict(shared)
        m["x"] = np.ascontiguousarray(xin[b])
        m["p"] = np.ascontiguousarray(p[l0:l0 + NL, b])
        m["positions"] = np.ascontiguousarray(pos[b].reshape(NT, 128))
        in_maps.append(m)
    res = run_bass_kernel_spmd(nc, in_maps, core_ids=list(range(8)))
    return np.stack([np.asarray(r["out"], dtype=np.float32) for r in res.results], axis=0)


def kernel(**inputs):
    x = np.asarray(inputs["x"], dtype=np.float32)
    if FUSED:
        return _run(x, inputs, [0, 1])
    x = _run(x, inputs, [0])
    x = _run(x, inputs, [1])
    return x
```
